# Optimizing a Trainium2 kernel written in Bass

```python
import math
import jax, jax.numpy as jnp
from jax import lax
import numpy as np


D_MODEL = 1024
BATCH = 8
SEQ = 2048
DEPTH = 2

SWA_Q_HEADS = 8
SWA_KV_HEADS = 2
SWA_HEAD_DIM = 64
SWA_WINDOW = 128
SWA_BLOCK = 128
ROPE_THETA = 500000.0
ROPE_DIM = SWA_HEAD_DIM // 4
DN_HEADS = 4
DN_HEAD_DIM = 128
DN_CONV = 4
DN_CHUNK = 64
POOL_WINDOWS = (2, 4, 8, 16)
POOL_GROUP = D_MODEL // 4
N_EXPERTS = 32
TOP_K = 4
D_EXPERT = D_MODEL
SWIGLU_LIMIT = 7.0
SWIGLU_ALPHA = 1.702
LN_EPS = 1e-5
RMS_EPS = 1e-6
DEEPNORM_ALPHA = (2 * DEPTH) ** 0.25
DEEPNORM_BETA = (8 * DEPTH) ** -0.25
SWA_Q_WIDTH = SWA_Q_HEADS * SWA_HEAD_DIM
SWA_KV_WIDTH = SWA_KV_HEADS * SWA_HEAD_DIM
DN_WIDTH = DN_HEADS * DN_HEAD_DIM
IN_SIZES = (SWA_Q_WIDTH, SWA_KV_WIDTH, SWA_KV_WIDTH, 3 * DN_WIDTH, DN_WIDTH, DN_HEADS, DN_HEADS)
IN_WIDTH = sum(IN_SIZES)
MIX_WIDTH = SWA_Q_WIDTH + DN_WIDTH
N_EVEN = (DEPTH + 1) // 2
N_ODD = DEPTH // 2

kernel_name = 'hybrid_swa_deltanet_pool_moe'


def split_cols(t, sizes):
    out, start = [], 0
    for s in sizes:
        out.append(t[..., start:start + s])
        start += s
    return out


def layer_norm(x, g, b):
    xf = x.astype(jnp.float32)
    mu = xf.mean(-1, keepdims=True)
    var = jnp.square(xf - mu).mean(-1, keepdims=True)
    return ((xf - mu) * lax.rsqrt(var + LN_EPS) * g.astype(jnp.float32) + b.astype(jnp.float32)).astype(x.dtype)


def partial_rotary(x, positions):
    half = ROPE_DIM // 2
    inv_freq = ROPE_THETA ** (-jnp.arange(half, dtype=jnp.float32) / half)
    ang = positions.astype(jnp.float32)[:, :, None] * inv_freq
    cos = jnp.cos(ang)[:, :, None, :]
    sin = jnp.sin(ang)[:, :, None, :]
    xr = x[..., :ROPE_DIM].astype(jnp.float32)
    x1, x2 = xr[..., :half], xr[..., half:]
    rot = jnp.concatenate([x1 * cos - x2 * sin, x2 * cos + x1 * sin], axis=-1).astype(x.dtype)
    return jnp.concatenate([rot, x[..., ROPE_DIM:]], axis=-1)


def sliding_window_attention(q, k, v, sinks):
    B, T = q.shape[0], q.shape[1]
    nb = T // SWA_BLOCK
    G = SWA_Q_HEADS // SWA_KV_HEADS
    qb = q.reshape(B, nb, SWA_BLOCK, SWA_KV_HEADS, G, SWA_HEAD_DIM).astype(jnp.float32)

    def band(t):
        tb = t.reshape(B, nb, SWA_BLOCK, SWA_KV_HEADS, SWA_HEAD_DIM)
        prev = jnp.pad(tb, ((0, 0), (1, 0), (0, 0), (0, 0), (0, 0)))[:, :-1]
        return jnp.concatenate([prev, tb], axis=2)

    kb = band(k).astype(jnp.float32)
    vb = band(v)
    s = jnp.einsum('bnqhgd,bnkhd->bnhgqk', qb, kb) * (SWA_HEAD_DIM ** -0.5)
    qi = jnp.arange(SWA_BLOCK)[:, None]
    kj = jnp.arange(2 * SWA_BLOCK)[None, :]
    rel = qi + SWA_BLOCK - kj
    in_window = (rel >= 0) & (rel < SWA_WINDOW)
    key_pos = jnp.arange(nb)[:, None, None] * SWA_BLOCK - SWA_BLOCK + kj[None]
    valid = in_window[None] & (key_pos >= 0)
    s = jnp.where(valid[None, :, None, None], s, -jnp.inf)
    sink = sinks.astype(jnp.float32).reshape(SWA_KV_HEADS, G)[None, None, :, :, None, None]
    m = jnp.maximum(s.max(-1, keepdims=True), sink)
    p = jnp.exp(s - m)
    p = p / (p.sum(-1, keepdims=True) + jnp.exp(sink - m))
    o = jnp.einsum('bnhgqk,bnkhd->bnqhgd', p.astype(vb.dtype), vb)
    return o.reshape(B, T, SWA_Q_WIDTH)


def causal_depthwise_conv(x, w):
    K = w.shape[0]
    return lax.conv_general_dilated(x, w[:, None, :].astype(x.dtype), window_strides=(1,),
                                    padding=((K - 1, 0),), dimension_numbers=('NWC', 'WIO', 'NWC'),
                                    feature_group_count=x.shape[-1])


def l2norm(x):
    return x * lax.rsqrt(jnp.sum(x * x, axis=-1, keepdims=True) + RMS_EPS)


def gated_delta_rule(q, k, v, g, beta):
    B, T, H, Dk = k.shape
    Dv = v.shape[-1]
    C = DN_CHUNK
    n = T // C

    def chunks(t):
        return t.reshape((B, n, C) + t.shape[2:]).swapaxes(2, 3)

    q, k, v, g, beta = chunks(q), chunks(k), chunks(v), chunks(g), chunks(beta)
    gc = jnp.cumsum(g, axis=-1)
    causal = jnp.tril(jnp.ones((C, C), dtype=bool))
    strict = jnp.tril(jnp.ones((C, C), dtype=bool), -1)
    decay = jnp.exp(jnp.where(causal, gc[..., :, None] - gc[..., None, :], -jnp.inf))
    kk = jnp.einsum('bnhid,bnhjd->bnhij', k, k)
    a_strict = jnp.where(strict, beta[..., :, None] * kk * decay, 0.0)
    rhs = jnp.concatenate([v * beta[..., None], k * (beta * jnp.exp(gc))[..., None]], axis=-1)
    sol = lax.linalg.triangular_solve(a_strict, rhs, left_side=True, lower=True, unit_diagonal=True)
    u, w = sol[..., :Dv], sol[..., Dv:]
    qk = jnp.einsum('bnhid,bnhjd->bnhij', q, k) * decay
    q_dec = q * jnp.exp(gc)[..., None]
    k_dec = k * jnp.exp(gc[..., -1:] - gc)[..., None]
    g_last = jnp.exp(gc[..., -1])

    def step(S, inp):
        w_c, u_c, q_c, k_c, qk_c, gl = inp
        v_new = u_c - jnp.einsum('bhcd,bhde->bhce', w_c, S)
        o = jnp.einsum('bhcd,bhde->bhce', q_c, S) + jnp.einsum('bhij,bhje->bhie', qk_c, v_new)
        S = S * gl[..., None, None] + jnp.einsum('bhcd,bhce->bhde', k_c, v_new)
        return S, o

    xs = (w.swapaxes(0, 1), u.swapaxes(0, 1), q_dec.swapaxes(0, 1), k_dec.swapaxes(0, 1),
          qk.swapaxes(0, 1), g_last.swapaxes(0, 1))
    S0 = jnp.zeros((B, H, Dk, Dv), jnp.float32)
    _, o = lax.scan(step, S0, xs)
    return o.transpose(1, 0, 3, 2, 4).reshape(B, T, H, Dv)


def mixer_swa_deltanet(x, positions, w_in, b_in, conv_w, a_log, dt_bias, norm_w, sinks, w_out, b_out):
    B, T, _ = x.shape
    proj = x @ w_in + b_in
    sq, sk, sv, dqkv, dz, da, db = split_cols(proj, IN_SIZES)
    q = partial_rotary(sq.reshape(B, T, SWA_Q_HEADS, SWA_HEAD_DIM), positions)
    k = partial_rotary(sk.reshape(B, T, SWA_KV_HEADS, SWA_HEAD_DIM), positions)
    v = sv.reshape(B, T, SWA_KV_HEADS, SWA_HEAD_DIM)
    a_out = sliding_window_attention(q, k, v, sinks)
    dqkv = jax.nn.silu(causal_depthwise_conv(dqkv, conv_w)).astype(jnp.float32)
    dq, dk, dv = split_cols(dqkv, (DN_WIDTH, DN_WIDTH, DN_WIDTH))
    dq = l2norm(dq.reshape(B, T, DN_HEADS, DN_HEAD_DIM)) * (DN_HEAD_DIM ** -0.5)
    dk = l2norm(dk.reshape(B, T, DN_HEADS, DN_HEAD_DIM))
    dv = dv.reshape(B, T, DN_HEADS, DN_HEAD_DIM)
    beta = jax.nn.sigmoid(db.astype(jnp.float32))
    g = -jnp.exp(a_log.astype(jnp.float32)) * jax.nn.softplus(da.astype(jnp.float32) + dt_bias.astype(jnp.float32))
    o = gated_delta_rule(dq, dk, dv, g, beta)
    z = dz.reshape(B, T, DN_HEADS, DN_HEAD_DIM).astype(jnp.float32)
    o = o * lax.rsqrt(jnp.mean(o * o, axis=-1, keepdims=True) + RMS_EPS) * norm_w.astype(jnp.float32) * jax.nn.silu(z)
    dn_out = o.reshape(B, T, DN_WIDTH).astype(x.dtype)
    return jnp.concatenate([a_out, dn_out], axis=-1) @ w_out + b_out


def multiscale_pool(x, pool_w, pool_b, pool_scale):
    B, T, D = x.shape
    xf = x.astype(jnp.float32)
    cs = jnp.pad(jnp.cumsum(xf, axis=1), ((0, 0), (1, 0), (0, 0)))
    t = jnp.arange(T)
    outs = []
    for gi, win in enumerate(POOL_WINDOWS):
        lo = jnp.maximum(t + 1 - win, 0)
        cnt = (t + 1 - lo).astype(jnp.float32)[None, :, None]
        csg = cs[..., gi * POOL_GROUP:(gi + 1) * POOL_GROUP]
        mean = (csg[:, 1:] - csg[:, lo]) / cnt
        outs.append(mean - xf[..., gi * POOL_GROUP:(gi + 1) * POOL_GROUP])
    pooled = jnp.stack(outs, axis=2).astype(x.dtype)
    y = jnp.einsum('btgc,gcd->btgd', pooled, pool_w) + pool_b
    return y.reshape(B, T, D) * pool_scale


def moe(x, router_w, router_b, w1, b1, w2, b2):
    B, T, D = x.shape
    xt = x.reshape(B * T, D)
    logits = (xt @ router_w + router_b).astype(jnp.float32)
    top_v, top_i = lax.top_k(logits, TOP_K)
    top_w = jax.nn.softmax(top_v, axis=-1)
    y = jnp.zeros((B * T, D), jnp.float32)
    for e in range(N_EXPERTS):
        gate = jnp.sum(jnp.where(top_i == e, top_w, 0.0), axis=-1)
        h = xt @ w1[e] + b1[e]
        h_glu = jnp.minimum(h[:, 0::2], SWIGLU_LIMIT)
        h_lin = jnp.clip(h[:, 1::2], -SWIGLU_LIMIT, SWIGLU_LIMIT)
        act = h_glu * jax.nn.sigmoid(SWIGLU_ALPHA * h_glu) * (h_lin + 1.0)
        y = y + gate[:, None] * (act @ w2[e] + b2[e]).astype(jnp.float32)
    return y.astype(x.dtype).reshape(B, T, D)


def setup_inputs(seed: int = 0) -> dict:
    key = jax.random.key(seed)
    ks = jax.random.split(key, 24)
    f32 = jnp.float32

    def nrm(k, shape, scale):
        return jax.random.normal(k, shape, f32) * scale

    x = jax.random.normal(ks[0], (BATCH, SEQ, D_MODEL), f32)
    offset = jax.random.randint(ks[1], (BATCH, 1), 0, 4096, dtype=jnp.int32)
    positions = offset + jnp.arange(SEQ, dtype=jnp.int32)[None, :]
    mix_w_in = nrm(ks[2], (N_EVEN, D_MODEL, IN_WIDTH), D_MODEL ** -0.5)
    mix_b_in = nrm(ks[3], (N_EVEN, IN_WIDTH), 0.01)
    dn_conv_w = nrm(ks[4], (N_EVEN, DN_CONV, 3 * DN_WIDTH), DN_CONV ** -0.5)
    dn_a_log = jnp.log(jax.random.uniform(ks[5], (N_EVEN, DN_HEADS), f32, 1.0, 16.0))
    dt = jnp.exp(jax.random.uniform(ks[6], (N_EVEN, DN_HEADS), f32, math.log(1e-3), math.log(1e-1)))
    dn_dt_bias = dt + jnp.log(-jnp.expm1(-dt))
    dn_norm_w = 1.0 + nrm(ks[7], (N_EVEN, DN_HEAD_DIM), 0.02)
    swa_sinks = nrm(ks[8], (N_EVEN, SWA_Q_HEADS), 1.0)
    mix_w_out = nrm(ks[9], (N_EVEN, MIX_WIDTH, D_MODEL), DEEPNORM_BETA * MIX_WIDTH ** -0.5)
    mix_b_out = nrm(ks[10], (N_EVEN, D_MODEL), 0.01)
    pool_w = nrm(ks[11], (N_ODD, len(POOL_WINDOWS), POOL_GROUP, POOL_GROUP), DEEPNORM_BETA * POOL_GROUP ** -0.5)
    pool_b = nrm(ks[12], (N_ODD, len(POOL_WINDOWS), POOL_GROUP), 0.01)
    pool_scale = 1.0 + nrm(ks[13], (N_ODD, D_MODEL), 0.02)
    ln1_g = 1.0 + nrm(ks[14], (DEPTH, D_MODEL), 0.02)
    ln1_b = nrm(ks[15], (DEPTH, D_MODEL), 0.01)
    router_w = nrm(ks[16], (DEPTH, D_MODEL, N_EXPERTS), D_MODEL ** -0.5)
    router_b = nrm(ks[17], (DEPTH, N_EXPERTS), 0.01)
    moe_w1 = nrm(ks[18], (DEPTH, N_EXPERTS, D_MODEL, 2 * D_EXPERT), D_MODEL ** -0.5)
    moe_b1 = nrm(ks[19], (DEPTH, N_EXPERTS, 2 * D_EXPERT), 0.01)
    moe_w2 = nrm(ks[20], (DEPTH, N_EXPERTS, D_EXPERT, D_MODEL), DEEPNORM_BETA * D_EXPERT ** -0.5)
    moe_b2 = nrm(ks[21], (DEPTH, N_EXPERTS, D_MODEL), 0.01)
    ln2_g = 1.0 + nrm(ks[22], (DEPTH, D_MODEL), 0.02)
    ln2_b = nrm(ks[23], (DEPTH, D_MODEL), 0.01)
    return {'x': x, 'positions': positions, 'mix_w_in': mix_w_in, 'mix_b_in': mix_b_in,
            'dn_conv_w': dn_conv_w, 'dn_a_log': dn_a_log, 'dn_dt_bias': dn_dt_bias, 'dn_norm_w': dn_norm_w,
            'swa_sinks': swa_sinks, 'mix_w_out': mix_w_out, 'mix_b_out': mix_b_out,
            'pool_w': pool_w, 'pool_b': pool_b, 'pool_scale': pool_scale,
            'ln1_g': ln1_g, 'ln1_b': ln1_b, 'router_w': router_w, 'router_b': router_b,
            'moe_w1': moe_w1, 'moe_b1': moe_b1, 'moe_w2': moe_w2, 'moe_b2': moe_b2,
            'ln2_g': ln2_g, 'ln2_b': ln2_b}


def reference(x, positions, mix_w_in, mix_b_in, dn_conv_w, dn_a_log, dn_dt_bias, dn_norm_w,
              swa_sinks, mix_w_out, mix_b_out, pool_w, pool_b, pool_scale,
              ln1_g, ln1_b, router_w, router_b, moe_w1, moe_b1, moe_w2, moe_b2, ln2_g, ln2_b):
    for layer in range(DEPTH):
        i = layer // 2
        if layer % 2 == 0:
            h = mixer_swa_deltanet(x, positions, mix_w_in[i], mix_b_in[i], dn_conv_w[i], dn_a_log[i],
                                   dn_dt_bias[i], dn_norm_w[i], swa_sinks[i], mix_w_out[i], mix_b_out[i])
        else:
            h = multiscale_pool(x, pool_w[i], pool_b[i], pool_scale[i])
        x = layer_norm(DEEPNORM_ALPHA * x + h, ln1_g[layer], ln1_b[layer])
        f = moe(x, router_w[layer], router_b[layer], moe_w1[layer], moe_b1[layer], moe_w2[layer], moe_b2[layer])
        x = layer_norm(DEEPNORM_ALPHA * x + f, ln2_g[layer], ln2_b[layer])
    return x
```

```python
from contextlib import ExitStack
import numpy as np
import concourse.bass as bass
import concourse.mybir as mybir

F32 = mybir.dt.float32
BF16 = mybir.dt.bfloat16
I32 = mybir.dt.int32
ALU = mybir.AluOpType
AF = mybir.ActivationFunctionType
AX = mybir.AxisListType

ENGS = ("pe", "act", "dve", "pool", "sp")
N_DMA_SEMS = 24
import os
SAME_ENG_SYNC = os.environ.get("SES", "1") == "1"


class Region:
    __slots__ = ("last_write", "readers")

    def __init__(self):
        self.last_write = None
        self.readers = []


class T:
    def __init__(self, ctx, name, handle, space, lo, hi):
        self.ctx, self.name, self.h, self.space, self.lo, self.hi = ctx, name, handle, space, lo, hi
        self.regions = {}
        self.pre = []
        self.dead = False

    def region(self, key):
        r = self.regions.get(key)
        if r is None:
            r = self.regions[key] = Region()
        return r

    def v(self, key, ap):
        if self.space == "ps":
            key = None
        return V(self, key, ap)

    def __getitem__(self, idx):
        return V(self, None, self.h[idx])


class V:
    __slots__ = ("t", "key", "ap")

    def __init__(self, t, key, ap):
        self.t, self.key, self.ap = t, key, ap


class Op:
    __slots__ = ("eng", "fn", "deps", "is_dma", "nparts", "dsem", "dval", "needs_inc", "ev", "idx", "extra_waits", "seq")

    def __init__(self, eng, fn):
        self.eng, self.fn = eng, fn
        self.deps = []
        self.is_dma = False
        self.nparts = 0
        self.dsem = None
        self.dval = 0
        self.needs_inc = False
        self.ev = None
        self.extra_waits = []


class Ctx:
    def __init__(self, nc):
        self.nc = nc
        self.ops = {e: [] for e in ENGS}
        self.tensors = []
        self.dma_rr = 0
        self.dma_rr_q = {}
        self.dma_counts = [0] * N_DMA_SEMS
        self.dma_last = [None] * N_DMA_SEMS
        self.sb_names = 0
        self.out_dma_ops = []
        self.seq = 0

    def sb(self, name, shape, dtype, off):
        esz = mybir.dt.size(dtype) if hasattr(mybir.dt, "size") else {F32: 4, BF16: 2, I32: 4}[dtype]
        nbytes = int(np.prod(shape[1:])) * esz
        self.sb_names += 1
        h = self.nc.alloc_sbuf_tensor_at(f"{name}_{self.sb_names}", list(shape), dtype, offset=off)
        t = T(self, name, h, "sb", off, off + nbytes)
        olds = []
        for o in self.tensors:
            if o.space == "sb" and o.lo < t.hi and t.lo < o.hi:
                o.dead = True
                olds.extend(o.pre)
                for r in o.regions.values():
                    if r.last_write is not None:
                        olds.append(r.last_write)
                    olds.extend(r.readers)
        best = {}
        for op in olds:
            k = ("d", op.dsem) if op.is_dma else ("e", op.eng)
            if k not in best or op.seq > best[k].seq:
                best[k] = op
        t.pre = list(best.values())
        self.tensors.append(t)
        return t

    def ps(self, name, shape, dtype=F32):
        h = self.nc.alloc_psum_tensor(name, list(shape), dtype)
        t = T(self, name, h, "ps", 0, 0)
        self.tensors.append(t)
        return t

    def _conflict_regions(self, v):
        t = v.t
        if v.key is None:
            regs = list(t.regions.values())
            if None not in t.regions:
                regs.append(t.region(None))
        else:
            regs = [t.region(v.key)]
            if None in t.regions:
                regs.append(t.regions[None])
        return regs

    def _record(self, op, reads, writes):
        deps = op.deps
        op.seq = self.seq
        self.seq += 1
        for v in list(reads) + list(writes):
            if isinstance(v, V):
                assert not v.t.dead, f"access to retired tensor {v.t.name}"
                if v.t.pre:
                    deps.extend(v.t.pre)
        writes = list(writes) + [v for v in reads if isinstance(v, V) and v.t.space == "ps"]
        for v in reads:
            if not isinstance(v, V):
                continue
            for r in self._conflict_regions(v):
                if r.last_write is not None:
                    deps.append(r.last_write)
        for v in writes:
            for r in self._conflict_regions(v):
                if r.last_write is not None:
                    deps.append(r.last_write)
                deps.extend(r.readers)
        for v in writes:
            own = v.t.region(v.key)
            for r in self._conflict_regions(v):
                r.readers = []
                if r is not own:
                    r.last_write = op
            own.last_write = op
        for v in reads:
            if isinstance(v, V):
                v.t.region(v.key).readers.append(op)
        self.ops[op.eng].append(op)

    def op(self, eng, fn, reads=(), writes=()):
        o = Op(eng, fn)
        self._record(o, reads, writes)
        return o

    def dma(self, eng, fns, reads=(), writes=(), is_output=False):
        o = Op(eng, fns)
        o.is_dma = True
        o.nparts = len(fns)
        lo, n = (8, N_DMA_SEMS - 8) if eng == "pool" else (0, 8)
        k = self.dma_rr_q.get(eng, 0)
        self.dma_rr_q[eng] = (k + 1) % n
        j = lo + k
        o.dsem = j
        if self.dma_last[j] is not None:
            o.deps.append(self.dma_last[j])
        self.dma_counts[j] += 16 * o.nparts
        o.dval = self.dma_counts[j]
        self.dma_last[j] = o
        self._record(o, reads, writes)
        if is_output:
            self.out_dma_ops.append(o)
        return o

    def emit(self):
        nc = self.nc
        for e in ENGS:
            for o in self.ops[e]:
                for d in o.deps:
                    if d.is_dma:
                        continue
                    if d.eng == o.eng and (d.eng == "pe" or not SAME_ENG_SYNC):
                        continue
                    d.needs_inc = True
        with ExitStack() as es:
            esems = {e: es.enter_context(nc.semaphore(f"s_{e}")) for e in ENGS}
            dsems = [es.enter_context(nc.semaphore(f"d_{j}")) for j in range(N_DMA_SEMS)]
            for e in ENGS:
                k = 0
                for o in self.ops[e]:
                    if o.is_dma:
                        o.ev = ("d", o.dsem, o.dval)
                    else:
                        if o.needs_inc:
                            k += 1
                            o.ev = ("e", e, k)
                        else:
                            o.ev = None
            self.n_waits = 0
            self.n_incs = 0

            def run(e, eng):
                waited = {}
                for o in self.ops[e]:
                    need = {}
                    for d in o.deps:
                        if not d.is_dma and d.eng == o.eng and (d.eng == "pe" or not SAME_ENG_SYNC):
                            continue
                        kind, s, val = d.ev
                        key = (kind, s)
                        if waited.get(key, 0) >= val:
                            continue
                        if need.get(key, 0) < val:
                            need[key] = val
                    for (kind, s), val in need.items():
                        sem = esems[s] if kind == "e" else dsems[s]
                        eng.wait_ge(sem, val)
                        waited[(kind, s)] = val
                        self.n_waits += 1
                    if o.is_dma:
                        for f in o.fn:
                            f(eng).then_inc(dsems[o.dsem], 16)
                    else:
                        ins = o.fn(eng)
                        if o.needs_inc:
                            ins.then_inc(esems[e], 1)
                            self.n_incs += 1
                if e == "sp":
                    for j in range(N_DMA_SEMS):
                        if self.dma_counts[j] > waited.get(("d", j), 0):
                            eng.wait_ge(dsems[j], self.dma_counts[j])

            with nc.Block() as block:
                @block.tensor
                def _(eng):
                    run("pe", eng)

                @block.scalar
                def _(eng):
                    run("act", eng)

                @block.vector
                def _(eng):
                    run("dve", eng)

                @block.gpsimd
                def _(eng):
                    run("pool", eng)

                @block.sync
                def _(eng):
                    run("sp", eng)

    def mm(self, out, lhsT, rhs, start=True, stop=True):
        return self.op("pe", lambda e: e.matmul(out.ap, lhsT.ap, rhs.ap, start=start, stop=stop),
                       reads=[lhsT, rhs] + ([] if start else [out]), writes=[out])

    def tr(self, out, in_, ident):
        return self.op("pe", lambda e: e.transpose(out.ap, in_.ap, ident.ap), reads=[in_, ident], writes=[out])

    def act(self, out, in_, func, bias=0.0, scale=1.0, accum_out=None, eng="act"):
        rd = [in_]
        b = bias.ap if isinstance(bias, V) else bias
        s = scale.ap if isinstance(scale, V) else scale
        if isinstance(bias, V):
            rd.append(bias)
        if isinstance(scale, V):
            rd.append(scale)
        wr = [out]
        kw = {}
        if accum_out is not None:
            wr.append(accum_out)
            kw["accum_out"] = accum_out.ap
        return self.op("act", lambda e: e.activation(out.ap, in_.ap, func, bias=b, scale=s, **kw), reads=rd, writes=wr)

    def ts(self, eng, out, in0, s1, op0, s2=None, op1=None, accum_out=None):
        rd = [in0]
        a1 = s1.ap if isinstance(s1, V) else s1
        a2 = s2.ap if isinstance(s2, V) else s2
        if isinstance(s1, V):
            rd.append(s1)
        if isinstance(s2, V):
            rd.append(s2)
        wr = [out]
        kw = {}
        if op1 is not None:
            kw["op1"] = op1
        if accum_out is not None:
            wr.append(accum_out)
            kw["accum_out"] = accum_out.ap
        return self.op(eng, lambda e: e.tensor_scalar(out.ap, in0.ap, a1, a2, op0, **kw), reads=rd, writes=wr)

    def tt(self, eng, out, in0, in1, op):
        return self.op(eng, lambda e: e.tensor_tensor(out.ap, in0.ap, in1.ap, op), reads=[in0, in1], writes=[out])

    def stt(self, eng, out, in0, scalar, in1, op0, op1, accum_out=None):
        rd = [in0, in1]
        a = scalar.ap if isinstance(scalar, V) else scalar
        if isinstance(scalar, V):
            rd.append(scalar)
        wr = [out]
        kw = {}
        if accum_out is not None:
            wr.append(accum_out)
            kw["accum_out"] = accum_out.ap
        return self.op(eng, lambda e: e.scalar_tensor_tensor(out.ap, in0.ap, a, in1.ap, op0, op1, **kw), reads=rd, writes=wr)

    def cp(self, eng, out, in_):
        if eng == "act":
            return self.op("act", lambda e: e.copy(out.ap, in_.ap), reads=[in_], writes=[out])
        return self.op(eng, lambda e: e.tensor_copy(out.ap, in_.ap), reads=[in_], writes=[out])

    def memset(self, eng, out, val):
        return self.op(eng, lambda e: e.memset(out.ap, val), reads=[], writes=[out])

    def load(self, eng, out, src_aps, dst_aps=None):
        if dst_aps is None:
            dst_aps = [out.ap]
        fns = [(lambda e, d=d, s=s: e.dma_start(out=d, in_=s)) for d, s in zip(dst_aps, src_aps)]
        return self.dma(eng, fns, reads=[], writes=[out])

    def store(self, eng, dst_aps, in_, src_aps=None):
        if src_aps is None:
            src_aps = [in_.ap]
        fns = [(lambda e, d=d, s=s: e.dma_start(out=d, in_=s)) for d, s in zip(dst_aps, src_aps)]
        return self.dma(eng, fns, reads=[in_], writes=[], is_output=True)


D = 1024
KC = 8
NCM = 1152
import os
PB0 = int(os.environ.get("PB0", "6"))
SB_BASE = 16512
SB_CAP = 229344
LN_EPS = 1e-5
ALPHA = 4 ** 0.25
LIMIT = 7.0
SW_ALPHA = 1.702


class Cfg:
    def __init__(self, T=2048, E=32, layers=(("mix", True), ("pool", True)), full_depth=2):
        self.T, self.E, self.layers = T, E, layers
        self.skip = set()
        self.lvl = 9
        self.dn_heads = 4
        self.ngrp = 2
        self.zf32 = False
        self.dumps = set()
        self.NT = T // 128
        self.NB = T // 512


class Alloc:
    def __init__(self, c):
        self.c = c
        self.off = SB_BASE
        self.marks = []

    def __call__(self, name, shape, dt):
        esz = 2 if dt == BF16 else 4
        n = int(np.prod(shape[1:])) * esz
        n = (n + 31) // 32 * 32
        assert self.off + n <= SB_CAP, f"SBUF overflow at {name}: {self.off + n - SB_CAP}"
        t = self.c.sb(name, shape, dt, self.off)
        self.off += n
        return t

    def mark(self):
        return self.off

    def reset(self, m):
        self.off = m


def run_interleaved(gens):
    gens = list(gens)
    while gens:
        for g in list(gens):
            try:
                next(g)
            except StopIteration:
                gens.remove(g)


def pipeline(items, nst, stage):
    for step in range(len(items) + nst - 1):
        for st in range(nst):
            i = step - st
            if 0 <= i < len(items):
                run_interleaved([stage(x, st) for x in items[i]])


def build(cfg):
    nc = bass.Bass("TRN2", target_bir_lowering=False)
    T, E, NT, NB = cfg.T, cfg.E, cfg.NT, cfg.NB
    L = len(cfg.layers)

    def din(name, shape, dt=F32):
        return nc.dram_tensor(name, list(shape), dt, kind="ExternalInput").ap()

    dr = {}
    dr["x"] = din("x", [T, D])
    dr["ident"] = din("ident", [128, 128])
    dr["ln1_g"] = din("ln1_g", [L, D])
    dr["ln1_b"] = din("ln1_b", [L, D])
    dr["ln2_g"] = din("ln2_g", [L, D])
    dr["ln2_b"] = din("ln2_b", [L, D])
    dr["router_w"] = din("router_w", [L, D, E])
    dr["router_b"] = din("router_b", [L, E])
    dr["moe_w1"] = din("moe_w1", [L, E, D, 2 * D])
    dr["moe_b1"] = din("moe_b1", [L, E, 2 * D])
    dr["moe_w2"] = din("moe_w2", [L, E, D, D])
    dr["moe_b2"] = din("moe_b2", [L, E, D])
    dr["bands"] = din("bands", [128, 12, 128])
    dr["pool_w"] = din("pool_w", [1, 4, 256, 256])
    dr["pool_b"] = din("pool_b", [1, D])
    dr["pool_scale"] = din("pool_scale", [1, D])
    dr["cmix"] = din("cmix", [128, NCM])
    dr["positions"] = din("positions", [1, T], I32)
    dr["mix_w_in"] = din("mix_w_in", [1, D, 2824])
    dr["mix_b_in"] = din("mix_b_in", [1, 2824])
    dr["dn_conv_w"] = din("dn_conv_w", [1, 4, 1536])
    dr["dn_a_log"] = din("dn_a_log", [1, 4])
    dr["dn_dt_bias"] = din("dn_dt_bias", [1, 4])
    dr["dn_norm_w"] = din("dn_norm_w", [1, 128])
    dr["swa_sinks"] = din("swa_sinks", [1, 8])
    dr["mix_w_out"] = din("mix_w_out", [1, D, D])
    dr["mix_b_out"] = din("mix_b_out", [1, D])
    out_d = nc.dram_tensor("out", [T, D], F32, kind="ExternalOutput").ap()

    c = Ctx(nc)
    A = Alloc(c)
    x_cur = A("x_cur", [128, NT, D], F32)
    xT = A("xT", [128, KC, T], BF16)
    ident = A("ident", [128, 128], F32)
    gates = A("gates", [128, NT, E], F32)
    PS = [c.ps(f"ps{i}", [128, 512], F32) for i in range(8)]

    dbg_outs = {}

    def dump(name, view, shape):
        if name not in cfg.dumps:
            return
        n = int(np.prod(shape[1:]))
        stg = c.sb("stg_" + name, [shape[0], n], F32, SB_CAP - 4 * n - 64)
        d = nc.dram_tensor("dbg_" + name, [shape[0], n], F32, kind="ExternalOutput").ap()
        c.cp("dve", stg[:, :], view)
        c.store("sp", [d[:, :]], stg[:, :])

    c.load("sp", ident[:, :], [dr["ident"][:, :]])
    xv = dr["x"].rearrange("(t p) d -> p t d", p=128)
    for tt in range(NT):
        c.load("sp", x_cur.v(tt, x_cur.h[:, tt, :]), [xv[:, tt, :]])

    def bcast_load(dst, src_row):
        c.load("sp", dst, [src_row.partition_broadcast(dst.ap.shape[0])])

    def layer_norm_all(g_bc, b_bc, small, after=None):
        st, mv, rstd = small
        NB_ = 10

        def stage(tt, sg):
            k = tt % NB_
            xt = x_cur.v(tt, x_cur.h[:, tt, :])
            stv = st.v(k, st.h[:, k * 12:(k + 1) * 12])
            mvv = mv.v(k, mv.h[:, 4 * k:4 * k + 2])
            rs = rstd.v(k, rstd.h[:, 2 * k:2 * k + 1])
            nm = rstd.v(k, rstd.h[:, 2 * k + 1:2 * k + 2])
            if sg == 0:
                for hf in range(2):
                    c.op("dve", lambda e, hf=hf: e.bn_stats(st.h[:, k * 12 + hf * 6:k * 12 + (hf + 1) * 6], x_cur.h[:, tt, hf * 512:(hf + 1) * 512]),
                         reads=[xt], writes=[stv])
                    yield
                c.op("dve", lambda e: e.bn_aggr(mv.h[:, 4 * k:4 * k + 2], st.h[:, k * 12:(k + 1) * 12]), reads=[stv], writes=[mvv])
                yield
                c.ts("dve", rs, mv.v(k, mv.h[:, 4 * k + 1:4 * k + 2]), LN_EPS, ALU.add)
                yield
            elif sg == 1:
                c.act(rs, rs, AF.Sqrt)
                yield
                c.op("dve", lambda e: e.reciprocal(rstd.h[:, 2 * k:2 * k + 1], rstd.h[:, 2 * k:2 * k + 1]), reads=[rs], writes=[rs])
                yield
                c.stt("dve", nm, mv.v(k, mv.h[:, 4 * k:4 * k + 1]), -1.0, rs, ALU.mult, ALU.mult)
                yield
            elif sg == 2:
                c.act(xt, xt, AF.Identity, bias=nm, scale=rs)
                yield
            else:
                c.tt("dve", xt, xt, g_bc[:, :], ALU.mult)
                yield
                c.tt("pool", xt, xt, b_bc[:, :], ALU.add)
                yield
                if after is not None:
                    after(tt)

        pipeline([tuple(range(t0, min(t0 + 2, NT))) for t0 in range(0, NT, 2)], 4, stage)

    def post_ln_all(l, rw32, rb_bc, b2s, bufs):
        NPB = len(bufs)

        def stage(tt, st):
            xT32, lg, sm, gTs = bufs[tt % NPB]
            m8, negm, ex, mask, ssum, rs = sm
            gt = gates.v(tt, gates.h[:, tt, :])
            if st == 0:
                for half in range(2):
                    pb = PS[(2 * tt + half) % 4]
                    for q in range(4):
                        kc = half * 4 + q
                        c.tr(pb.v(q, pb.h[:, q * 128:(q + 1) * 128]), x_cur.v(tt, x_cur.h[:, tt, kc * 128:(kc + 1) * 128]), ident[:, :])
                        yield
                    pv = V(pb, None, pb.h[:, :].rearrange("p (q n) -> p q n", q=4))
                    c.cp("act", xT.v(tt, xT.h[:, half * 4:(half + 1) * 4, tt * 128:(tt + 1) * 128]), pv)
                    yield
                    c.cp("dve", xT32.v(half, xT32.h[:, half * 4:(half + 1) * 4, :]), pv)
                    yield
            elif st == 1:
                lgp = PS[4 + tt % 2]
                for kc in range(KC):
                    c.mm(lgp.v(None, lgp.h[:, 0:E]), xT32.v(kc // 4, xT32.h[:, kc, :]), rw32.v(None, rw32.h[:, kc, :]),
                         start=(kc == 0), stop=(kc == KC - 1))
                    yield
                c.tt("dve", lg[:, :], lgp.v(None, lgp.h[:, 0:E]), rb_bc[:, :], ALU.add)
                yield
                c.op("dve", lambda e: e.max(m8.h[:, :], lg.h[:, :]), reads=[lg[:, :]], writes=[m8[:, :]])
                yield
                c.ts("dve", negm[:, :], m8.v(None, m8.h[:, 0:1]), -1.0, ALU.mult)
                yield
            elif st == 2:
                c.act(ex[:, :], lg[:, :], AF.Exp, bias=negm[:, 0:1], scale=1.0)
                yield
                c.ts("dve", mask[:, :], lg[:, :], m8.v(None, m8.h[:, 3:4]), ALU.is_ge)
                yield
                c.tt("dve", ex[:, :], ex[:, :], mask[:, :], ALU.mult)
                yield
                c.op("dve", lambda e: e.reduce_sum(ssum.h[:, :], ex.h[:, :], AX.X), reads=[ex[:, :]], writes=[ssum[:, :]])
                yield
                c.op("dve", lambda e: e.reciprocal(rs.h[:, :], ssum.h[:, :]), reads=[ssum[:, :]], writes=[rs[:, :]])
                yield
                c.ts("dve", gt, ex[:, :], rs[:, 0:1], ALU.mult)
                yield
            else:
                gp = PS[6 + tt % 2]
                c.tr(gp.v(None, gp.h[0:E, 0:128]), gt, ident[:, :])
                yield
                c.cp("act", gTs[:, :], gp.v(None, gp.h[0:E, 0:128]))
                yield
                for half in range(2):
                    pb = PS[(2 * tt + half) % 4]
                    c.mm(pb[:, :], gTs[:, :], b2s.v(None, b2s.h[:, half * 512:(half + 1) * 512]))
                    yield
                    xh = x_cur.v(tt, x_cur.h[:, tt, half * 512:(half + 1) * 512])
                    c.stt("dve", xh, xh, ALPHA, pb[:, :], ALU.mult, ALU.add)
                    yield

        pipeline([tuple(range(t0, min(t0 + 2, NT))) for t0 in range(0, NT, 2)], 4, stage)

    NBUF = 3

    def moe_load_unit(l, u, w1u, w2u):
        e, q = u // 4, u % 4
        b = u % NBUF
        src1 = dr["moe_w1"][l, e].rearrange("(kc p) n -> p kc n", p=128)
        c.load("pool", w1u[b][:, :, :], [src1[:, :, 512 * q:512 * q + 512]])
        src2 = dr["moe_w2"][l, e, 256 * q:256 * q + 256, :].rearrange("(j p) n -> p j n", p=128)
        c.load("pool", w2u[b][:, :, :], [src2])

    def moe(l, m0, w1u, w2u):
        A.reset(m0)
        CAP = SW_ALPHA * LIMIT / (1.0 + float(np.exp(-SW_ALPHA * LIMIT)))
        actT = [A(f"actT{i}", [128, 2, T], BF16) for i in range(2)]
        NTMP = 3
        tsg = [A(f"tsg{i}", [128, 512], F32) for i in range(NTMP)]
        tlr = [A(f"tlr{i}", [128, 512], F32) for i in range(NTMP)]
        b1s = A("b1s", [E, 2 * D], F32)
        b1T = A("b1T", [128, 16, E], F32)
        c.load("sp", b1s[:, :], [dr["moe_b1"][l, :, :]])
        pb = PS[6]
        for cc in range(16):
            j, gl = cc // 2, cc % 2
            c.tr(pb.v(cc, pb.h[:, cc * E:(cc + 1) * E]), b1s.v(None, b1s.h[:, 256 * j + gl:256 * j + 256:2]),
                 ident.v(None, ident.h[0:E, 0:E]))
        c.cp("dve", b1T[:, :, :], V(pb, None, pb.h[:, 0:16 * E].rearrange("p (c e) -> p c e", c=16)))
        bgv = b1T.v(None, b1T.h[:, 0:16:2, :])
        blv = b1T.v(None, b1T.h[:, 1:16:2, :])
        c.ts("dve", bgv, bgv, SW_ALPHA, ALU.mult)
        c.ts("dve", blv, blv, 1.0, ALU.add, 1.0 / SW_ALPHA, ALU.mult)

        units = [(e, q) for e in range(E) for q in range(4)]
        w1v = dr["moe_w1"]
        w2v = dr["moe_w2"]

        def load_unit(u):
            moe_load_unit(l, u, w1u, w2u)

        cnt = [0]
        ycnt = [0]
        pendF = []

        def h_group(u, tb, j2, gl):
            e, q = units[u]
            b = u % NBUF
            j = 2 * q + j2
            i = cnt[0] % NTMP
            hp = PS[(cnt[0] % 2) + 2 * gl]
            for kc in range(KC):
                c.mm(hp[:, :], w1u[b].v(None, w1u[b].h[:, kc, 256 * j2 + gl:256 * j2 + 256:2]),
                     xT.v(None, xT.h[:, kc, tb * 512:(tb + 1) * 512]), start=(kc == 0), stop=(kc == KC - 1))
            if gl == 0:
                c.act(tsg[i][:, :], hp[:, :], AF.Silu, bias=b1T.v(None, b1T.h[:, 2 * j, e:e + 1]), scale=SW_ALPHA)
            else:
                c.act(tlr[i][:, :], hp[:, :], AF.Identity, bias=b1T.v(None, b1T.h[:, 2 * j + 1, e:e + 1]), scale=1.0 / SW_ALPHA)
                c.ts("pool", tlr[i][:, :], tlr[i][:, :], (LIMIT + 1.0) / SW_ALPHA, ALU.min, (1.0 - LIMIT) / SW_ALPHA, ALU.max)
                ab = u % 2
                dst = actT[ab].v((j2, tb), actT[ab].h[:, j2, tb * 512:(tb + 1) * 512])
                pendF.append((dst, tsg[i], tlr[i]))
                cnt[0] += 1

        def flushF(keep):
            while len(pendF) > keep:
                dst, a_, b_ = pendF.pop(0)
                c.stt("dve", dst, a_[:, :], CAP, b_[:, :], ALU.min, ALU.mult)

        def y_groups(u, tb, gs):
            e, q = units[u]
            b = u % NBUF
            ab = u % 2
            for g in gs:
                tt = tb * 4 + g // 2
                half = g % 2
                yp = PS[4 + ycnt[0] % 4]
                ycnt[0] += 1
                for j2 in range(2):
                    c.mm(yp[:, :], actT[ab].v((j2, tb), actT[ab].h[:, j2, tt * 128:(tt + 1) * 128]),
                         w2u[b].v(None, w2u[b].h[:, j2, half * 512:(half + 1) * 512]), start=(j2 == 0), stop=(j2 == 1))
                xh = x_cur.v(tt, x_cur.h[:, tt, half * 512:(half + 1) * 512])
                c.stt("dve", xh, yp[:, :], gates.v(tt, gates.h[:, tt, e:e + 1]), xh, ALU.mult, ALU.add)

        NU = len(units)
        for u in range(NU + 1):
            for tb in range(NB):
                k = 0
                for j2 in range(2):
                    for gl in range(2):
                        if u < NU:
                            h_group(u, tb, j2, gl)
                        if u > 0:
                            y_groups(u - 1, tb, (2 * k, 2 * k + 1))
                        k += 1
                        if k == 2:
                            flushF(1 if u < NU else 0)
                        if k == 4:
                            flushF(1 if (u < NU and tb < NB - 1) else 0)
            if u + 2 < NU:
                load_unit(u + 2)
        flushF(0)

    def pool_mixer(i):
        bands = A("bands", [128, 12, 128], F32)
        pw = A("pw", [128, 4, 2, 256], BF16)
        pbb = A("pbb", [128, D], F32)
        psb = A("psb", [128, D], F32)
        pooledT = A("pooledT", [128, KC, T], BF16)
        tmp = [A(f"ptmp{k}", [128, 512], F32) for k in range(2)]
        c.load("sp", bands[:, :, :], [dr["bands"][:, :, :]])
        for g in range(4):
            c.load("pool", pw.v(g, pw.h[:, g, :, :]), [dr["pool_w"][i, g].rearrange("(kc p) n -> p kc n", p=128)])
        bcast_load(pbb[:, :], dr["pool_b"][i:i + 1, :])
        bcast_load(psb[:, :], dr["pool_scale"][i:i + 1, :])
        k = 0
        for n in range(NT):
            for half in range(2):
                pb = PS[k % 2]
                k += 1
                for q in range(4):
                    cc = half * 4 + q
                    g = cc // 2
                    o = pb.v(None, pb.h[:, q * 128:(q + 1) * 128])
                    cur = x_cur.v(n, x_cur.h[:, n, cc * 128:(cc + 1) * 128])
                    if n == 0:
                        c.mm(o, cur, bands.v(None, bands.h[:, 3 * g + 0, :]))
                    else:
                        prev = x_cur.v(n - 1, x_cur.h[:, n - 1, cc * 128:(cc + 1) * 128])
                        c.mm(o, prev, bands.v(None, bands.h[:, 3 * g + 2, :]), start=True, stop=False)
                        c.mm(o, cur, bands.v(None, bands.h[:, 3 * g + 1, :]), start=False, stop=True)
                c.cp("act" if half == 0 else "dve", pooledT.v(n, pooledT.h[:, half * 4:(half + 1) * 4, n * 128:(n + 1) * 128]),
                     V(pb, None, pb.h[:, :].rearrange("p (q n) -> p q n", q=4)))
        tmp4 = tmp + [A(f"ptmpx{k2}", [128, 512], F32) for k2 in range(2)]

        def pool_out(item, st_):
            n, half = item
            idx_ = 2 * n + half
            pb = PS[2 + idx_ % 4]
            tm = tmp4[idx_ % 4]
            for gg in range(2):
                g = half * 2 + gg
                for kc in range(2):
                    c.mm(pb.v(None, pb.h[:, gg * 256:(gg + 1) * 256]), pooledT.v(n, pooledT.h[:, 2 * g + kc, n * 128:(n + 1) * 128]),
                         pw.v(g, pw.h[:, g, kc, :]), start=(kc == 0), stop=(kc == 1))
            yield
            sl = slice(half * 512, (half + 1) * 512)
            c.tt("dve", tm[:, :], pb[:, :], pbb.v(None, pbb.h[:, sl]), ALU.add)
            yield
            c.tt("dve", tm[:, :], tm[:, :], psb.v(None, psb.h[:, sl]), ALU.mult)
            yield
            xh = x_cur.v(n, x_cur.h[:, n, sl])
            c.stt("dve", xh, xh, ALPHA, tm[:, :], ALU.mult, ALU.add)
            yield

        pipeline([((n, 0), (n, 1)) for n in range(NT)], 1, pool_out)

    def mixer0(i):
        PI = float(np.pi)
        psrr = [0]

        def nb():
            b = PS[psrr[0] % 8]
            psrr[0] += 1
            return b

        b_in = dr["mix_b_in"]
        w_in = dr["mix_w_in"][i].rearrange("(kc p) n -> p kc n", p=128)
        w_outv = dr["mix_w_out"][i]
        cm = A("cm", [128, NCM], F32)
        c.load("sp", cm[:, :], [dr["cmix"][:, :]])
        Uc = cm.v(None, cm.h[:, 0:128])
        ones = cm.v(None, cm.h[:, 128:256])
        Prot = cm.v(None, cm.h[:, 256:384])
        mlow = cm.v(None, cm.h[:, 384:512])
        mupi = cm.v(None, cm.h[:, 512:640])
        swam = cm.h[:, 640:896]
        freq = cm.v(None, cm.h[:, 896:897])
        id32 = cm.v(None, cm.h[:, 897:1025])
        identb = A("identb", [128, 128], BF16)
        c.cp("dve", identb[:, :], id32)
        binT = A("binT", [128, 22], F32)
        bkd = A("bkd", [128, 2], F32)
        for kv in range(2):
            src = b_in[i, 512 + 64 * kv:576 + 64 * kv].rearrange("(p o) -> p o", o=1)
            c.load("sp", bkd.v(None, bkd.h[0:64, kv:kv + 1]), [src])
            c.load("sp", bkd.v(None, bkd.h[64:128, kv:kv + 1]), [src])
        bv_bc = A("bv_bc", [128, 128], F32)
        bab_bc = A("bab_bc", [128, 8], F32)
        alog_bc = A("alog_bc", [128, 4], F32)
        dtb_bc = A("dtb_bc", [128, 4], F32)
        nw_bc = A("nw_bc", [128, 128], F32)
        sink_bc = A("sink_bc", [128, 8], F32)
        bcast_load(bv_bc[:, :], b_in[i:i + 1, 640:768])
        bcast_load(bab_bc[:, :], b_in[i:i + 1, 2816:2824])
        bcast_load(alog_bc[:, :], dr["dn_a_log"][i:i + 1, :])
        bcast_load(dtb_bc[:, :], dr["dn_dt_bias"][i:i + 1, :])
        bcast_load(nw_bc[:, :], dr["dn_norm_w"][i:i + 1, :])
        bcast_load(sink_bc[:, :], dr["swa_sinks"][i:i + 1, :])
        cwT = A("cwT", [128, 12, 4], F32)
        m_small = A.mark()
        bout_bc = A("bout_bc", [128, D], F32)
        bcast_load(bout_bc[:, :], dr["mix_b_out"][i:i + 1, :])
        bin22 = A("bin22", [22, 128], F32)
        c.load("sp", bin22[:, :], [b_in[i, 0:2816].rearrange("(c p) -> c p", p=128)])
        pb = nb()
        c.tr(pb.v(None, pb.h[:, 0:22]), bin22[:, :], id32.t.v(None, cm.h[0:22, 897:897 + 22]))
        c.cp("dve", binT[:, :], pb.v(None, pb.h[:, 0:22]))
        cw4 = A("cw4", [4, 1536], F32)
        c.load("sp", cw4[:, :], [dr["dn_conv_w"][i, :, :]])
        pb = nb()
        for cc in range(12):
            c.tr(pb.v(None, pb.h[:, cc * 4:(cc + 1) * 4]), cw4.v(None, cw4.h[:, cc * 128:(cc + 1) * 128]),
                 id32.t.v(None, cm.h[0:4, 897:901]))
        c.cp("dve", cwT[:, :, :], V(pb, None, pb.h[:, 0:48].rearrange("p (c j) -> p c j", c=12)))
        for tt in range(NT):
            xt = x_cur.v(tt, x_cur.h[:, tt, :])
            c.stt("dve", xt, xt, ALPHA, bout_bc[:, :], ALU.mult, ALU.add)
        A.reset(m_small)

        if "swa" not in cfg.skip:
            w_q = A("w_q", [128, KC, 512], BF16)
            w_kd = A("w_kd", [128, KC, 2, 128], BF16)
            w_v = A("w_v", [128, KC, 128], BF16)
            w_oA = A("w_oA", [128, 4, D], BF16)
            c.load("pool", w_q[:, :, :], [w_in[:, :, 0:512]])
            for kv in range(2):
                for rep in range(2):
                    c.load("pool", w_kd.v(None, w_kd.h[:, :, kv, rep * 64:(rep + 1) * 64]), [w_in[:, :, 512 + 64 * kv:576 + 64 * kv]])
            c.load("pool", w_v[:, :, :], [w_in[:, :, 640:768]])
            c.load("pool", w_oA[:, :, :], [w_outv[0:512, :].rearrange("(kc p) n -> p kc n", p=128)])
            qT = A("qT", [128, 4, T], BF16)
            kTd = A("kTd", [128, 2, T], BF16)
            vtok = A("vtok", [128, NT, 2, 192], BF16)
            c.memset("pool", vtok[:, :, :, :], 0.0)
            aoT = A("aoT", [128, 4, T], BF16)
            m_rot = A.mark()
            posi = A("posi", [128, 512], I32)
            ang = A("ang", [128, 512], F32)
            Sn = A("Sn", [128, 512], F32)
            Cn = A("Cn", [128, 512], F32)
            qtmp = [A(f"qtmp{k}", [128, 512], F32) for k in range(2)]
            t1 = [A(f"t1{k}", [128, 512], F32) for k in range(2)]
            uu = [A(f"uu{k}", [128, 512], F32) for k in range(2)]
            kf = A("kf", [128, 512], F32)
            ki = A("ki", [128, 512], I32)
            rk = [0]

            def rot_block(ps_src, bias_v, SC, dst):
                k = rk[0] % 2
                rk[0] += 1
                c.act(qtmp[k][:, :], ps_src[:, :], AF.Identity, bias=bias_v)
                pw_ = nb()
                c.mm(pw_[:, :], Prot, qtmp[k][:, :])
                c.tt("dve", t1[k][:, :], qtmp[k][:, :], Cn[:, :], ALU.mult)
                c.stt("dve", uu[k][:, :], pw_[:, :], SC, Sn[:, :], ALU.mult, ALU.mult)
                c.stt("dve", dst, t1[k][:, :], SC, uu[k][:, :], ALU.mult, ALU.add)

            for tb in range(NB):
                bs = slice(tb * 512, (tb + 1) * 512)
                c.load("sp", posi[:, :], [dr["positions"][0:1, bs].partition_broadcast(128)])
                c.cp("dve", ang[:, :], posi[:, :])
                c.ts("dve", ang[:, :], ang[:, :], freq, ALU.mult)
                for tab, shift in ((Sn, 0.0), (Cn, PI / 2)):
                    c.ts("dve", tab[:, :], ang[:, :], shift, ALU.add)
                    c.ts("dve", kf[:, :], tab[:, :], 1.0 / (2 * PI), ALU.mult)
                    c.cp("dve", ki[:, :], kf[:, :])
                    c.cp("dve", kf[:, :], ki[:, :])
                    c.stt("dve", tab[:, :], kf[:, :], -6.28125, tab[:, :], ALU.mult, ALU.add)
                    c.stt("dve", tab[:, :], kf[:, :], -(2 * PI - 6.28125), tab[:, :], ALU.mult, ALU.add)
                    c.ts("dve", tab[:, :], tab[:, :], PI, ALU.min, -PI, ALU.max)
                    c.act(tab[:, :], tab[:, :], AF.Sin)
                for ch in range(4):
                    pq = nb()
                    for kc in range(KC):
                        c.mm(pq[:, :], w_q.v(None, w_q.h[:, kc, ch * 128:(ch + 1) * 128]), xT.v(None, xT.h[:, kc, bs]),
                             start=(kc == 0), stop=(kc == KC - 1))
                    rot_block(pq, binT.v(None, binT.h[:, ch:ch + 1]), 0.125, qT.v((ch, tb), qT.h[:, ch, bs]))
                for kv in range(2):
                    pk = nb()
                    for kc in range(KC):
                        c.mm(pk[:, :], w_kd.v(None, w_kd.h[:, kc, kv, :]), xT.v(None, xT.h[:, kc, bs]),
                             start=(kc == 0), stop=(kc == KC - 1))
                    rot_block(pk, bkd.v(None, bkd.h[:, kv:kv + 1]), 1.0, kTd.v((kv, tb), kTd.h[:, kv, bs]))
            for tt in range(NT):
                pv_ = nb()
                for kc in range(KC):
                    c.mm(pv_.v(None, pv_.h[:, 0:128]), xT.v(None, xT.h[:, kc, tt * 128:(tt + 1) * 128]), w_v.v(None, w_v.h[:, kc, :]),
                         start=(kc == 0), stop=(kc == KC - 1))
                c.tt("dve", vtok.v(None, vtok.h[:, tt, :, 64:128]), V(pv_, None, pv_.h[:, 0:128].rearrange("p (a b) -> p a b", a=2)),
                     V(bv_bc, None, bv_bc.h[:, :].rearrange("p (a b) -> p a b", a=2)), ALU.add)
            dump("qT", qT.v(None, qT.h[:, 0, 0:512]), [128, 512])
            dump("kT", kTd.v(None, kTd.h[:, 0, 0:512]), [128, 512])
            dump("Sn", Sn[:, :], [128, 512])
            dump("Cn", Cn[:, :], [128, 512])
            dump("vtok", vtok.v(None, vtok.h[:, 0, 0, :]), [128, 192])
            dump("x0", x_cur.v(None, x_cur.h[:, 0, 0:512]), [128, 512])
            A.reset(m_rot)
            NS = 9
            smb = [A(f"smb{k}", [128, 256], F32) for k in range(NS)]
            pex = [A(f"pex{k}", [128, 256], F32) for k in range(NS)]
            pnb = [A(f"pnb{k}", [128, 256], BF16) for k in range(NS)]
            pTs = [A(f"pTs{k}", [128, 2, 128], BF16) for k in range(NS)]
            sc1 = [A(f"sc1{k}", [128, 8], F32) for k in range(NS)]
            its = [(n, ch, hh) for n in range(NT) for ch in range(4) for hh in range(2)]
            po_of = {}

            def att_stage(idx, st):
                n, ch, hh = its[idx]
                k = idx % NS
                kv = ch // 2
                h = 2 * ch + hh
                k0 = 0 if n > 0 else 128
                NK = 256 - k0
                nkb = NK // 128
                keys = slice(n * 128 - 128 + k0, (n + 1) * 128)
                rows = slice(64 * hh, 64 * hh + 64)
                s1 = sc1[k]
                col = lambda a_: s1.v(None, s1.h[:, a_:a_ + 1])
                sm_ = smb[k].v(None, smb[k].h[:, 0:NK])
                pe_ = pex[k].v(None, pex[k].h[:, 0:NK])
                pn_ = pnb[k].v(None, pnb[k].h[:, 0:NK])
                if st == 0:
                    pa = nb()
                    c.mm(pa.v(None, pa.h[:, 0:NK]), qT.v((ch, n // 4), qT.h[rows, ch, n * 128:(n + 1) * 128]),
                         kTd.v(None, kTd.h[rows, kv, keys]))
                    yield
                    c.tt("dve", sm_, pa.v(None, pa.h[:, 0:NK]), cm.v(None, swam[:, k0:256]), ALU.add)
                    yield
                    c.op("dve", lambda e: e.reduce_max(s1.h[:, 0:1], smb[k].h[:, 0:NK], AX.X), reads=[sm_], writes=[col(0)])
                    yield
                    c.tt("dve", col(1), col(0), sink_bc.v(None, sink_bc.h[:, h:h + 1]), ALU.max)
                    yield
                    c.ts("dve", col(2), col(1), -1.0, ALU.mult)
                    yield
                elif st == 1:
                    c.act(pe_, sm_, AF.Exp, bias=col(2), accum_out=col(3))
                    yield
                    c.act(col(4), col(2), AF.Exp, bias=sink_bc.v(None, sink_bc.h[:, h:h + 1]))
                    yield
                    c.tt("dve", col(5), col(3), col(4), ALU.add)
                    yield
                    c.op("dve", lambda e: e.reciprocal(s1.h[:, 6:7], s1.h[:, 5:6]), reads=[col(5)], writes=[col(6)])
                    yield
                    c.act(pn_, pe_, AF.Copy, scale=col(6))
                    yield
                elif st == 2:
                    ptp = nb()
                    ptb = ptp.h[:, :].bitcast(BF16)
                    for kb in range(nkb):
                        c.tr(V(ptp, None, ptb[:, kb * 128:(kb + 1) * 128]), pnb[k].v(None, pnb[k].h[:, kb * 128:(kb + 1) * 128]), identb[:, :])
                        yield
                    c.cp("act", pTs[k].v(None, pTs[k].h[:, 0:nkb, :]), V(ptp, None, ptb[:, 0:nkb * 128].rearrange("p (a b) -> p a b", a=nkb)))
                    yield
                else:
                    if hh == 0:
                        po_of[(n, ch)] = nb()
                    po = po_of[(n, ch)]
                    for kb in range(nkb):
                        ktile = n - (nkb - 1) + kb
                        vsl = slice(64, 192) if hh == 0 else slice(0, 128)
                        c.mm(po.v(None, po.h[:, 0:128]), vtok.v(None, vtok.h[:, ktile, kv, vsl]),
                             pTs[k].v(None, pTs[k].h[:, kb, :]), start=(hh == 0 and kb == 0), stop=(hh == 1 and kb == nkb - 1))
                        yield
                    if hh == 1:
                        c.cp("dve", aoT.v((ch, n), aoT.h[:, ch, n * 128:(n + 1) * 128]), po.v(None, po.h[:, 0:128]))
                        yield

            pipeline([(i2, i2 + 1) for i2 in range(0, len(its), 2)], 4, att_stage)
            dump("aoT", aoT.v(None, aoT.h[:, 0, 0:512]), [128, 512])
            for tt in range(NT):
                for half in range(2):
                    py = nb()
                    for ch in range(4):
                        c.mm(py[:, :], aoT.v((ch, tt), aoT.h[:, ch, tt * 128:(tt + 1) * 128]),
                             w_oA.v(None, w_oA.h[:, ch, half * 512:(half + 1) * 512]), start=(ch == 0), stop=(ch == 3))
                    xh = x_cur.v(tt, x_cur.h[:, tt, half * 512:(half + 1) * 512])
                    c.tt("dve", xh, xh, py[:, :], ALU.add)

        if "dn" in cfg.skip:
            return
        A.reset(m_small)
        bz_bc = A("bz_bc", [128, 512], F32)
        bcast_load(bz_bc[:, :], b_in[i:i + 1, 2304:2816])
        madd_low = A("madd_low", [128, 512], F32)
        madd_upi = A("madd_upi", [128, 512], F32)
        id4 = A("id4", [128, 512], F32)
        for r4 in range(4):
            sl = slice(r4 * 128, (r4 + 1) * 128)
            c.ts("dve", madd_low.v(None, madd_low.h[:, sl]), mlow, 30000.0, ALU.mult, -30000.0, ALU.add)
            c.ts("dve", madd_upi.v(None, madd_upi.h[:, sl]), mupi, 30000.0, ALU.mult, -30000.0, ALU.add)
            c.cp("dve", id4.v(None, id4.h[:, sl]), id32)
        w_ab = A("w_ab", [128, KC, 8], BF16)
        c.load("pool", w_ab[:, :, :], [w_in[:, :, 2816:2824]])
        nea = A("nea", [128, 4], F32)
        c.act(nea[:, :], alog_bc[:, :], AF.Exp)
        c.ts("dve", nea[:, :], nea[:, :], -1.0, ALU.mult)
        tabs = {nm: A(nm, [128, NT, 4], F32) for nm in ("gcs", "ngc", "eg", "egl", "ekd", "beta", "nbeta", "nebg")}
        aball = A("aball", [128, NT, 8], F32)
        gtmp = [A(f"gtmp{k}", [128, NT, 4], F32) for k in range(4)]
        glall = A("glall", [128, NT, 4], F32)
        NTB = NT // 4 if NT >= 4 else 1
        for g4 in range(0, NT, 4):
            pab = nb()
            for q4 in range(min(4, NT - g4)):
                tt = g4 + q4
                for kc in range(KC):
                    c.mm(pab.v(None, pab.h[:, q4 * 8:(q4 + 1) * 8]), xT.v(None, xT.h[:, kc, tt * 128:(tt + 1) * 128]), w_ab.v(None, w_ab.h[:, kc, :]),
                         start=(kc == 0), stop=(kc == KC - 1))
            n4 = min(4, NT - g4)
            for q4 in range(n4):
                c.tt("dve", aball.v(None, aball.h[:, g4 + q4, :]), pab.v(None, pab.h[:, q4 * 8:(q4 + 1) * 8]), bab_bc[:, :], ALU.add)
        T4 = lambda nm: tabs[nm][:, :, :]
        da_v = aball.v(None, aball.h[:, :, 0:4])
        db_v = aball.v(None, aball.h[:, :, 4:8])
        c.act(gtmp[0][:, :, :], db_v, AF.Exp, scale=-1.0)
        c.ts("dve", gtmp[0][:, :, :], gtmp[0][:, :, :], 1.0, ALU.add)
        c.op("dve", lambda e: e.reciprocal(tabs["beta"].h[:, :, :], gtmp[0].h[:, :, :]), reads=[gtmp[0][:, :, :]], writes=[T4("beta")])
        c.ts("dve", T4("nbeta"), T4("beta"), -1.0, ALU.mult)
        for tt in range(NT):
            c.tt("dve", gtmp[1].v(None, gtmp[1].h[:, tt, :]), aball.v(None, aball.h[:, tt, 0:4]), dtb_bc[:, :], ALU.add)
        c.ts("dve", gtmp[2][:, :, :], gtmp[1][:, :, :], -1.0, ALU.mult)
        c.tt("dve", gtmp[2][:, :, :], gtmp[2][:, :, :], gtmp[1][:, :, :], ALU.max)
        c.act(gtmp[2][:, :, :], gtmp[2][:, :, :], AF.Exp, scale=-1.0)
        c.act(gtmp[2][:, :, :], gtmp[2][:, :, :], AF.Ln, bias=1.0)
        c.stt("dve", gtmp[3][:, :, :], gtmp[1][:, :, :], 0.0, gtmp[2][:, :, :], ALU.max, ALU.add)
        for tt in range(NT):
            c.tt("dve", gtmp[3].v(None, gtmp[3].h[:, tt, :]), gtmp[3].v(None, gtmp[3].h[:, tt, :]), nea[:, :], ALU.mult)
        pg = nb()
        for tt in range(NT):
            c.mm(pg.v(None, pg.h[:, tt * 8:tt * 8 + 4]), Uc, gtmp[3].v(None, gtmp[3].h[:, tt, :]))
            c.mm(pg.v(None, pg.h[:, tt * 8 + 4:tt * 8 + 8]), ones, gtmp[3].v(None, gtmp[3].h[:, tt, :]))
        pgv = pg.h[:, 0:NT * 8].rearrange("p (t e) -> p t e", e=8)
        c.cp("dve", T4("gcs"), V(pg, None, pgv[:, :, 0:4]))
        c.cp("dve", glall[:, :, :], V(pg, None, pgv[:, :, 4:8]))
        c.ts("dve", T4("ngc"), T4("gcs"), -1.0, ALU.mult)
        c.act(T4("eg"), T4("gcs"), AF.Exp)
        c.act(T4("egl"), glall[:, :, :], AF.Exp)
        c.tt("dve", gtmp[0][:, :, :], glall[:, :, :], T4("gcs"), ALU.subtract)
        c.act(T4("ekd"), gtmp[0][:, :, :], AF.Exp)
        c.stt("dve", T4("nebg"), T4("beta"), -1.0, T4("eg"), ALU.mult, ALU.mult)
        m_dn = A.mark()

        def tcol(nm, tt, h):
            return tabs[nm].v(tt, tabs[nm].h[:, tt, h:h + 1])

        for h in range(4):
            if h >= cfg.dn_heads:
                break
            A.reset(m_dn)
            w_h = A("w_h", [128, KC, 3, 128], BF16)
            w_z = A("w_z", [128, KC, 128], BF16)
            w_oB = A("w_oB", [128, D], BF16)
            for ct in range(3):
                c0 = 768 + 512 * ct + 128 * h
                c.load("pool", w_h.v(ct, w_h.h[:, :, ct, :]), [w_in[:, :, c0:c0 + 128]])
            c.load("pool", w_z[:, :, :], [w_in[:, :, 2304 + 128 * h:2432 + 128 * h]])
            c.load("pool", w_oB[:, :], [w_outv[512 + 128 * h:640 + 128 * h, :]])
            qTn = A("qTn", [128, T], F32)
            kTn = A("kTn", [128, T], F32)
            kdec = A("kdec", [128, NT, 128], F32)
            vb = A("vb", [128, NT, 128], F32)
            TT = A("TT", [128, T], F32)
            QKT = A("QKT", [128, T], F32)
            Sst = A("Sst", [128, 128], F32)
            dnT2 = [A(f"dnT{k}", [128, 128], BF16) for k in range(2)]
            zall = A("zall", [128, NT, 128], F32 if cfg.zf32 else BF16)
            zb = [A(f"zb{k}", [128, 128], F32) for k in range(2)]
            for tt in range(NT):
                pz = nb()
                for kc in range(KC):
                    c.mm(pz.v(None, pz.h[:, 0:128]), xT.v(None, xT.h[:, kc, tt * 128:(tt + 1) * 128]), w_z.v(None, w_z.h[:, kc, :]),
                         start=(kc == 0), stop=(kc == KC - 1))
                c.tt("dve", zb[tt % 2][:, :], pz.v(None, pz.h[:, 0:128]), bz_bc.v(None, bz_bc.h[:, 128 * h:128 * h + 128]), ALU.add)
                c.act(zall.v(tt, zall.h[:, tt, :]), zb[tt % 2][:, :], AF.Silu)
            smalls = [A(f"dsm{k}", [128, 4], F32) for k in range(2)]
            tok = [A(f"tok{k}", [128, 128], F32) for k in range(2)]
            junk = A("junk", [128, 128], F32)
            m_conv = A.mark()
            pre = A("pre", [128, T + 4], F32)
            cbuf = A("cbuf", [128, T], F32)
            sqb = [A(f"sqb{k}", [128, 512], F32) for k in range(4)]
            c.memset("dve", pre.v("pad", pre.h[:, 0:3]), 0.0)
            kk_ = [0]
            pend_eps = []
            for ct in range(3):
                cidx = ct * 4 + h
                for tb in range(NB):
                    bs = slice(tb * 512, (tb + 1) * 512)
                    pp = nb()
                    for kc in range(KC):
                        c.mm(pp[:, :], w_h.v(ct, w_h.h[:, kc, ct, :]), xT.v(None, xT.h[:, kc, bs]), start=(kc == 0), stop=(kc == KC - 1))
                    c.act(pre.v(tb, pre.h[:, 3 + tb * 512:3 + (tb + 1) * 512]), pp[:, :], AF.Identity, bias=binT.v(None, binT.h[:, 6 + cidx:7 + cidx]))
                for tb in range(NB):
                    cb = cbuf.v(tb, cbuf.h[:, tb * 512:(tb + 1) * 512])
                    prev_dep = [pre.v(tb - 1, pre.h[:, 0:1])] if tb > 0 else [pre.v("pad", pre.h[:, 0:1])]
                    w3 = cwT.v(None, cwT.h[:, cidx, 3:4])
                    c.op("dve", lambda e, tb=tb, w3=w3: e.tensor_scalar(cbuf.h[:, tb * 512:(tb + 1) * 512], pre.h[:, 3 + tb * 512:3 + (tb + 1) * 512], w3.ap, None, ALU.mult),
                         reads=[pre.v(tb, pre.h[:, 0:1]), w3], writes=[cb])
                    for j in (2, 1, 0):
                        wj = cwT.v(None, cwT.h[:, cidx, j:j + 1])
                        c.op("dve", lambda e, tb=tb, j=j, wj=wj: e.scalar_tensor_tensor(cbuf.h[:, tb * 512:(tb + 1) * 512], pre.h[:, j + tb * 512:j + (tb + 1) * 512],
                                                                                      wj.ap, cbuf.h[:, tb * 512:(tb + 1) * 512], ALU.mult, ALU.add),
                             reads=[pre.v(tb, pre.h[:, 0:1]), wj, cb] + prev_dep, writes=[cb])
                    c.act(cb, cb, AF.Silu)
                    if ct < 2:
                        sq = sqb[tb % 4]
                        c.act(sq[:, :], cb, AF.Square)
                        pss = nb()
                        c.mm(pss[:, :], ones, sq[:, :])
                        if pend_eps:
                            sq_, pss_ = pend_eps.pop()
                            c.ts("dve", sq_[:, :], pss_[:, :], 1e-6, ALU.add)
                        pend_eps.append((sq, pss))
                if ct < 2:
                    sq_, pss_ = pend_eps.pop()
                    c.ts("dve", sq_[:, :], pss_[:, :], 1e-6, ALU.add)
                    for tb in range(NB):
                        sq = sqb[tb % 4]
                        c.act(sq[:, :], sq[:, :], AF.Sqrt)
                        c.op("dve", lambda e, sq=sq: e.reciprocal(sq.h[:, :], sq.h[:, :]), reads=[sq[:, :]], writes=[sq[:, :]])
                        dstT = qTn if ct == 0 else kTn
                        for t4 in range(4):
                            tt = tb * 4 + t4
                            c.stt("dve", dstT.v(tt, dstT.h[:, tt * 128:(tt + 1) * 128]), V(cbuf, tb, cbuf.h[:, tt * 128:(tt + 1) * 128]),
                                  (128.0 ** -0.5) if ct == 0 else 1.0, sq.v(None, sq.h[:, t4 * 128:(t4 + 1) * 128]), ALU.mult, ALU.mult)
                if ct == 0:
                    continue
                for tt in range(NT):
                    ts_ = slice(tt * 128, (tt + 1) * 128)
                    ptk = nb()
                    pt_ = ptk.v(None, ptk.h[:, 0:128])
                    if ct == 2:
                        c.tr(pt_, cbuf.v(tt // 4, cbuf.h[:, ts_]), id32)
                        c.ts("dve", vb.v(tt, vb.h[:, tt, :]), pt_, tcol("beta", tt, h), ALU.mult)
                    else:
                        c.tr(pt_, kTn.v(tt, kTn.h[:, ts_]), id32)
                        c.act(kdec.v(tt, kdec.h[:, tt, :]), pt_, AF.Copy, scale=tcol("ekd", tt, h))
            A.reset(m_conv)
            NG = NT // 4
            NGRP = cfg.ngrp
            for g0 in range(0, NG, NGRP):
                A.reset(m_conv)
                grp = [g for g in range(g0, g0 + NGRP) if g < NG]
                Pb = {g: [A(f"P{g % NGRP}_{k}", [128, 512], F32) for k in range(2)] for g in grp}
                PTb = {g: [A(f"PT{g % NGRP}_{k}", [128, 512], F32) for k in range(2)] for g in grp}
                dgb = {g: Pb[g][1] for g in grp}
                dcl = {g: PTb[g][1] for g in grp}
                dcu = {g: A(f"dcu{g % NGRP}", [128, 512], F32) for g in grp}
                for g in grp:
                    gs = slice(g * 512, (g + 1) * 512)
                    pkk, pqk, pR = nb(), nb(), nb()
                    for q4 in range(4):
                        tt = g * 4 + q4
                        ts_ = slice(tt * 128, (tt + 1) * 128)
                        qs = slice(q4 * 128, (q4 + 1) * 128)
                        c.ts("dve", dgb[g].v(None, dgb[g].h[:, qs]), id32, tcol("gcs", tt, h), ALU.mult)
                        c.mm(pkk.v(None, pkk.h[:, qs]), kTn.v(tt, kTn.h[:, ts_]), kTn.v(tt, kTn.h[:, ts_]))
                        c.mm(pqk.v(None, pqk.h[:, qs]), kTn.v(tt, kTn.h[:, ts_]), qTn.v(tt, qTn.h[:, ts_]))
                        c.mm(pR.v(None, pR.h[:, qs]), ones, dgb[g].v(None, dgb[g].h[:, qs]))
                    c.stt("dve", dcl[g][:, :], pR[:, :], -1.0, madd_low[:, :], ALU.mult, ALU.add)
                    c.tt("dve", dcu[g][:, :], pR[:, :], madd_upi[:, :], ALU.add)
                    for q4 in range(4):
                        tt = g * 4 + q4
                        qs = slice(q4 * 128, (q4 + 1) * 128)
                        c.act(dcl[g].v(None, dcl[g].h[:, qs]), dcl[g].v(None, dcl[g].h[:, qs]), AF.Exp, bias=tcol("gcs", tt, h))
                        c.act(dcu[g].v(None, dcu[g].h[:, qs]), dcu[g].v(None, dcu[g].h[:, qs]), AF.Exp, bias=tcol("ngc", tt, h))
                        c.stt("dve", Pb[g][0].v(None, Pb[g][0].h[:, qs]), pkk.v(None, pkk.h[:, qs]), tcol("nbeta", tt, h),
                              dcl[g].v(None, dcl[g].h[:, qs]), ALU.mult, ALU.mult)
                    c.tt("dve", QKT.v(g, QKT.h[:, gs]), pqk[:, :], dcu[g][:, :], ALU.mult)
                    pbt = nb()
                    for q4 in range(4):
                        qs = slice(q4 * 128, (q4 + 1) * 128)
                        c.tr(pbt.v(None, pbt.h[:, qs]), Pb[g][0].v(None, Pb[g][0].h[:, qs]), id32)
                    c.cp("act", PTb[g][0][:, :], pbt[:, :])
                    c.tt("dve", TT.v(g, TT.h[:, gs]), PTb[g][0][:, :], id4[:, :], ALU.add)
                def sq_level(lev):
                    a, b2 = (lev - 1) % 2, lev % 2
                    outs = {}
                    for g in grp:
                        pP = nb()
                        for q4 in range(4):
                            qs = slice(q4 * 128, (q4 + 1) * 128)
                            c.mm(pP.v(None, pP.h[:, qs]), PTb[g][a].v(None, PTb[g][a].h[:, qs]), Pb[g][a].v(None, Pb[g][a].h[:, qs]))
                        pPT = None
                        if lev < 6:
                            pPT = nb()
                            for q4 in range(4):
                                qs = slice(q4 * 128, (q4 + 1) * 128)
                                c.mm(pPT.v(None, pPT.h[:, qs]), Pb[g][a].v(None, Pb[g][a].h[:, qs]), PTb[g][a].v(None, PTb[g][a].h[:, qs]))
                        outs[g] = (pP, pPT)
                    for g in grp:
                        pP, pPT = outs[g]
                        c.cp("act", Pb[g][b2][:, :], pP[:, :])
                        if pPT is not None:
                            c.cp("dve", PTb[g][b2][:, :], pPT[:, :])

                def t_level(lev):
                    b2 = lev % 2
                    outs = {}
                    for g in grp:
                        pT_ = nb()
                        for q4 in range(4):
                            qs = slice(q4 * 128, (q4 + 1) * 128)
                            tsl = slice(g * 512 + q4 * 128, g * 512 + (q4 + 1) * 128)
                            c.mm(pT_.v(None, pT_.h[:, qs]), Pb[g][b2].v(None, Pb[g][b2].h[:, qs]), TT.v(g, TT.h[:, tsl]))
                        outs[g] = pT_
                    for g in grp:
                        gs = slice(g * 512, (g + 1) * 512)
                        c.tt("dve", TT.v(g, TT.h[:, gs]), TT.v(g, TT.h[:, gs]), outs[g][:, :], ALU.add)

                sq_level(1)
                for lev in range(2, 7):
                    sq_level(lev)
                    t_level(lev - 1)
                t_level(6)
            A.reset(m_conv)
            rr = [A(f"rr{k}", [128, 128], F32) for k in range(2)]
            vn = [A(f"vn{k}", [128, 128], F32) for k in range(2)]
            qs_ = [A(f"qs{k}", [128, 128], F32) for k in range(2)]
            ob = [A(f"ob{k}", [128, 128], F32) for k in range(2)]
            osm = [A(f"osm{k}", [128, 4], F32) for k in range(2)]
            c.memset("dve", Sst[:, :], 0.0)

            def scan_R(tt):
                k = tt % 2
                ts_ = slice(tt * 128, (tt + 1) * 128)
                p1 = nb()
                c.mm(p1.v(None, p1.h[:, 0:128]), kTn.v(tt, kTn.h[:, ts_]), Sst[:, :])
                yield
                c.stt("dve", rr[k][:, :], p1.v(None, p1.h[:, 0:128]), tcol("nebg", tt, h), vb.v(tt, vb.h[:, tt, :]), ALU.mult, ALU.add)
                yield
                p2 = nb()
                c.mm(p2.v(None, p2.h[:, 0:128]), TT.v(tt // 4, TT.h[:, ts_]), rr[k][:, :])
                yield
                c.cp("act", vn[k][:, :], p2.v(None, p2.h[:, 0:128]))
                yield
                p3 = nb()
                c.mm(p3.v(None, p3.h[:, 0:128]), qTn.v(tt, qTn.h[:, ts_]), Sst[:, :])
                yield
                c.act(qs_[k][:, :], p3.v(None, p3.h[:, 0:128]), AF.Copy, scale=tcol("eg", tt, h))
                yield
                p5 = nb()
                c.mm(p5.v(None, p5.h[:, 0:128]), kdec.v(tt, kdec.h[:, tt, :]), vn[k][:, :])
                yield
                c.stt("dve", Sst[:, :], Sst[:, :], tcol("egl", tt, h), p5.v(None, p5.h[:, 0:128]), ALU.mult, ALU.add)
                yield

            def scan_O(tt):
                k = tt % 2
                ts_ = slice(tt * 128, (tt + 1) * 128)
                p4 = nb()
                c.mm(p4.v(None, p4.h[:, 0:128]), QKT.v(tt // 4, QKT.h[:, ts_]), vn[k][:, :])
                yield
                c.tt("dve", ob[k][:, :], p4.v(None, p4.h[:, 0:128]), qs_[k][:, :], ALU.add)
                yield
                o1 = osm[k]
                c.act(junk[:, :], ob[k][:, :], AF.Square, accum_out=o1.v(None, o1.h[:, 0:1]))
                yield
                c.ts("dve", o1.v(None, o1.h[:, 1:2]), o1.v(None, o1.h[:, 0:1]), 1.0 / 128.0, ALU.mult, 1e-6, ALU.add)
                yield
                c.act(o1.v(None, o1.h[:, 1:2]), o1.v(None, o1.h[:, 1:2]), AF.Sqrt)
                yield
                c.op("dve", lambda e, o1=o1: e.reciprocal(o1.h[:, 2:3], o1.h[:, 1:2]),
                     reads=[o1.v(None, o1.h[:, 1:2])], writes=[o1.v(None, o1.h[:, 2:3])])
                yield
                c.stt("dve", ob[k][:, :], ob[k][:, :], o1.v(None, o1.h[:, 2:3]), nw_bc[:, :], ALU.mult, ALU.mult)
                yield
                c.tt("dve", ob[k][:, :], ob[k][:, :], zall.v(tt, zall.h[:, tt, :]), ALU.mult)
                yield
                p6 = nb()
                c.tr(p6.v(None, p6.h[:, 0:128]), ob[k][:, :], id32)
                yield
                c.cp("act", dnT2[k][:, :], p6.v(None, p6.h[:, 0:128]))
                yield
                for half in range(2):
                    py = nb()
                    c.mm(py[:, :], dnT2[k][:, :], w_oB.v(None, w_oB.h[:, half * 512:(half + 1) * 512]))
                    yield
                    xh = x_cur.v(tt, x_cur.h[:, tt, half * 512:(half + 1) * 512])
                    c.tt("dve", xh, xh, py[:, :], ALU.add)
                    yield

            for tt in range(NT + 1):
                gens = []
                if tt < NT:
                    gens.append(scan_R(tt))
                if tt >= 1:
                    gens.append(scan_O(tt - 1))
                run_interleaved(gens)

    m_persist = A.mark()
    for l, (mixer, do_moe) in enumerate(cfg.layers):
        A.reset(m_persist)
        if mixer == "pool":
            pool_mixer(l // 2)
        elif mixer == "mix":
            for tt in range(NT):
                for half in range(2):
                    pb = PS[(2 * tt + half) % 8]
                    for q in range(4):
                        kc = half * 4 + q
                        c.tr(pb.v(None, pb.h[:, q * 128:(q + 1) * 128]), x_cur.v(tt, x_cur.h[:, tt, kc * 128:(kc + 1) * 128]), ident[:, :])
                    c.cp("act" if half == 0 else "dve", xT.v(tt, xT.h[:, half * 4:(half + 1) * 4, tt * 128:(tt + 1) * 128]),
                         V(pb, None, pb.h[:, :].rearrange("p (q n) -> p q n", q=4)))
            mixer0(l // 2)
        A.reset(m_persist)
        g_bc = A("g_bc", [128, D], F32)
        b_bc = A("b_bc", [128, D], F32)
        st = A("st", [128, 120], F32)
        mv = A("mv", [128, 40], F32)
        rstd = A("rstd", [128, 20], F32)
        m_mix = A.mark()
        if mixer != "none":
            bcast_load(g_bc[:, :], dr["ln1_g"][l:l + 1, :])
            bcast_load(b_bc[:, :], dr["ln1_b"][l:l + 1, :])
            layer_norm_all(g_bc, b_bc, (st, mv, rstd))
        A.reset(m_mix)
        w1u = [A(f"w1u{i_}", [128, KC, 512], BF16) for i_ in range(NBUF)]
        w2u = [A(f"w2u{i_}", [128, 2, D], BF16) for i_ in range(NBUF)]
        if "moe" not in cfg.skip:
            moe_load_unit(l, 0, w1u, w2u)
            moe_load_unit(l, 1, w1u, w2u)
        m_moe = A.mark()
        rw32 = A("rw32", [128, KC, E], F32)
        rb_bc = A("rb_bc", [128, E], F32)
        b2s = A("b2s", [E, D], F32)
        bufs = []
        for kb_ in range(10):
            bufs.append((A(f"xT32_{kb_}", [128, KC, 128], F32), A(f"lg{kb_}", [128, E], F32),
                         (A(f"m8_{kb_}", [128, 8], F32), A(f"negm{kb_}", [128, 1], F32), A(f"ex{kb_}", [128, E], F32), A(f"mask{kb_}", [128, E], F32),
                          A(f"ssum{kb_}", [128, 1], F32), A(f"rs{kb_}", [128, 1], F32)), A(f"gTs{kb_}", [E, 128], F32)))
        c.load("sp", rw32[:, :, :], [dr["router_w"][l].rearrange("(kc p) e -> p kc e", p=128)])
        bcast_load(rb_bc[:, :], dr["router_b"][l:l + 1, :])
        c.load("sp", b2s[:, :], [dr["moe_b2"][l, :, :]])
        if "post" not in cfg.skip:
            post_ln_all(l, rw32, rb_bc, b2s, bufs)
        if "moe" not in cfg.skip:
            moe(l, m_moe, w1u, w2u)
        bcast_load(g_bc[:, :], dr["ln2_g"][l:l + 1, :])
        bcast_load(b_bc[:, :], dr["ln2_b"][l:l + 1, :])
        last = (l == len(cfg.layers) - 1)
        ov = out_d.rearrange("(t p) d -> p t d", p=128)
        st_fn = (lambda tt: c.store("sp", [ov[:, tt, :]], x_cur.v(tt, x_cur.h[:, tt, :]))) if last else None
        if "ln2" not in cfg.skip:
            layer_norm_all(g_bc, b_bc, (st, mv, rstd), after=st_fn)
        elif last:
            for tt in range(NT):
                st_fn(tt)
    c.emit()
    return nc, c


def host_consts():
    p = np.arange(128)
    ident = np.eye(128, dtype=np.float32)
    bands = np.zeros((128, 12, 128), np.float32)
    s = p[:, None]
    t = p[None, :]
    for g, w in enumerate((2, 4, 8, 16)):
        d = t - s
        cur = np.where((d >= 0) & (d < w), 1.0 / w, 0.0) - (d == 0)
        cnt = np.minimum(t + 1, w)
        cur0 = np.where((d >= 0) & (d < w), 1.0 / cnt, 0.0) - (d == 0)
        dprev = t + 128 - s
        prev = np.where(dprev < w, 1.0 / w, 0.0)
        bands[:, 3 * g + 0, :] = cur0
        bands[:, 3 * g + 1, :] = cur
        bands[:, 3 * g + 2, :] = prev
    cm = np.zeros((128, NCM), np.float32)
    cm[:, 0:128] = (s <= t)
    cm[:, 128:256] = 1.0
    P = np.zeros((128, 128), np.float32)
    for base in (0, 64):
        for m in range(8):
            P[base + m + 8, base + m] = -1.0
        for m in range(8, 16):
            P[base + m - 8, base + m] = 1.0
    cm[:, 256:384] = P
    cm[:, 384:512] = (s > t)
    cm[:, 512:640] = (t >= s)
    qi = p[:, None]
    kj = np.arange(256)[None, :]
    valid = np.where(kj < 128, kj > qi, (kj - 128) <= qi)
    cm[:, 640:896] = np.where(valid, 0.0, -30000.0)
    half = 8
    inv = 500000.0 ** (-np.arange(half, dtype=np.float32) / half)
    f = np.zeros(128, np.float32)
    for base in (0, 64):
        for d in range(16):
            f[base + d] = inv[d % 8]
    cm[:, 896] = f
    cm[:, 897:1025] = ident
    return dict(ident=ident, bands=bands, cmix=cm)


_CACHE = {}


def kernel(**inputs):
    from concourse.bass_utils import run_bass_kernel_spmd
    B = inputs["x"].shape[0]
    if "nc" not in _CACHE:
        cfg = Cfg(T=2048, E=32, layers=(("mix", True), ("pool", True)))
        _CACHE["nc"] = build(cfg)[0]
    nc = _CACHE["nc"]
    hc = host_consts()
    f32 = lambda a: np.ascontiguousarray(np.asarray(a), dtype=np.float32)
    shared = {k: f32(inputs[k]) for k in ("ln1_g", "ln1_b", "ln2_g", "ln2_b", "router_w", "router_b", "moe_w1", "moe_b1",
                                           "moe_w2", "moe_b2", "pool_w", "pool_scale", "mix_w_in", "mix_b_in", "dn_conv_w",
                                           "dn_a_log", "dn_dt_bias", "dn_norm_w", "swa_sinks", "mix_w_out", "mix_b_out")}
    shared["pool_b"] = f32(inputs["pool_b"]).reshape(1, D)
    shared.update(hc)
    x = f32(inputs["x"])
    pos = np.ascontiguousarray(np.asarray(inputs["positions"]), dtype=np.int32)
    in_maps = []
    for b in range(B):
        m = dict(shared)
        m["x"] = x[b]
        m["positions"] = pos[b:b + 1]
        in_maps.append(m)
    res = run_bass_kernel_spmd(nc, in_maps, core_ids=list(range(B)))
    return np.stack([np.asarray(r["out"], dtype=np.float32) for r in res.results], axis=0)
```

```python
from contextlib import ExitStack
import numpy as np
import concourse.bass as bass
import concourse.mybir as mybir

F32 = mybir.dt.float32
BF16 = mybir.dt.bfloat16
I32 = mybir.dt.int32
ALU = mybir.AluOpType
AF = mybir.ActivationFunctionType
AX = mybir.AxisListType

ENGS = ("pe", "act", "dve", "pool", "sp")
N_DMA_SEMS = 24
import os
SAME_ENG_SYNC = os.environ.get("SES", "1") == "1"


class Region:
    __slots__ = ("last_write", "readers")

    def __init__(self):
        self.last_write = None
        self.readers = []


class T:
    def __init__(self, ctx, name, handle, space, lo, hi):
        self.ctx, self.name, self.h, self.space, self.lo, self.hi = ctx, name, handle, space, lo, hi
        self.regions = {}
        self.pre = []
        self.dead = False

    def region(self, key):
        r = self.regions.get(key)
        if r is None:
            r = self.regions[key] = Region()
        return r

    def v(self, key, ap):
        if self.space == "ps":
            key = None
        return V(self, key, ap)

    def __getitem__(self, idx):
        return V(self, None, self.h[idx])


class V:
    __slots__ = ("t", "key", "ap")

    def __init__(self, t, key, ap):
        self.t, self.key, self.ap = t, key, ap


class Op:
    __slots__ = ("eng", "fn", "deps", "is_dma", "nparts", "dsem", "dval", "needs_inc", "ev", "idx", "extra_waits", "seq")

    def __init__(self, eng, fn):
        self.eng, self.fn = eng, fn
        self.deps = []
        self.is_dma = False
        self.nparts = 0
        self.dsem = None
        self.dval = 0
        self.needs_inc = False
        self.ev = None
        self.extra_waits = []


class Ctx:
    def __init__(self, nc):
        self.nc = nc
        self.ops = {e: [] for e in ENGS}
        self.tensors = []
        self.dma_rr = 0
        self.dma_rr_q = {}
        self.dma_counts = [0] * N_DMA_SEMS
        self.dma_last = [None] * N_DMA_SEMS
        self.sb_names = 0
        self.out_dma_ops = []
        self.seq = 0

    def sb(self, name, shape, dtype, off):
        esz = mybir.dt.size(dtype) if hasattr(mybir.dt, "size") else {F32: 4, BF16: 2, I32: 4}[dtype]
        nbytes = int(np.prod(shape[1:])) * esz
        self.sb_names += 1
        h = self.nc.alloc_sbuf_tensor_at(f"{name}_{self.sb_names}", list(shape), dtype, offset=off)
        t = T(self, name, h, "sb", off, off + nbytes)
        olds = []
        for o in self.tensors:
            if o.space == "sb" and o.lo < t.hi and t.lo < o.hi:
                o.dead = True
                olds.extend(o.pre)
                for r in o.regions.values():
                    if r.last_write is not None:
                        olds.append(r.last_write)
                    olds.extend(r.readers)
        best = {}
        for op in olds:
            k = ("d", op.dsem) if op.is_dma else ("e", op.eng)
            if k not in best or op.seq > best[k].seq:
                best[k] = op
        t.pre = list(best.values())
        self.tensors.append(t)
        return t

    def ps(self, name, shape, dtype=F32):
        h = self.nc.alloc_psum_tensor(name, list(shape), dtype)
        t = T(self, name, h, "ps", 0, 0)
        self.tensors.append(t)
        return t

    def _conflict_regions(self, v):
        t = v.t
        if v.key is None:
            regs = list(t.regions.values())
            if None not in t.regions:
                regs.append(t.region(None))
        else:
            regs = [t.region(v.key)]
            if None in t.regions:
                regs.append(t.regions[None])
        return regs

    def _record(self, op, reads, writes):
        deps = op.deps
        op.seq = self.seq
        self.seq += 1
        for v in list(reads) + list(writes):
            if isinstance(v, V):
                assert not v.t.dead, f"access to retired tensor {v.t.name}"
                if v.t.pre:
                    deps.extend(v.t.pre)
        writes = list(writes) + [v for v in reads if isinstance(v, V) and v.t.space == "ps"]
        for v in reads:
            if not isinstance(v, V):
                continue
            for r in self._conflict_regions(v):
                if r.last_write is not None:
                    deps.append(r.last_write)
        for v in writes:
            for r in self._conflict_regions(v):
                if r.last_write is not None:
                    deps.append(r.last_write)
                deps.extend(r.readers)
        for v in writes:
            own = v.t.region(v.key)
            for r in self._conflict_regions(v):
                r.readers = []
                if r is not own:
                    r.last_write = op
            own.last_write = op
        for v in reads:
            if isinstance(v, V):
                v.t.region(v.key).readers.append(op)
        self.ops[op.eng].append(op)

    def op(self, eng, fn, reads=(), writes=()):
        o = Op(eng, fn)
        self._record(o, reads, writes)
        return o

    def dma(self, eng, fns, reads=(), writes=(), is_output=False):
        o = Op(eng, fns)
        o.is_dma = True
        o.nparts = len(fns)
        lo, n = (8, N_DMA_SEMS - 8) if eng == "pool" else (0, 8)
        k = self.dma_rr_q.get(eng, 0)
        self.dma_rr_q[eng] = (k + 1) % n
        j = lo + k
        o.dsem = j
        if self.dma_last[j] is not None:
            o.deps.append(self.dma_last[j])
        self.dma_counts[j] += 16 * o.nparts
        o.dval = self.dma_counts[j]
        self.dma_last[j] = o
        self._record(o, reads, writes)
        if is_output:
            self.out_dma_ops.append(o)
        return o

    def emit(self):
        nc = self.nc
        for e in ENGS:
            for o in self.ops[e]:
                for d in o.deps:
                    if d.is_dma:
                        continue
                    if d.eng == o.eng and (d.eng == "pe" or not SAME_ENG_SYNC):
                        continue
                    d.needs_inc = True
        with ExitStack() as es:
            esems = {e: es.enter_context(nc.semaphore(f"s_{e}")) for e in ENGS}
            dsems = [es.enter_context(nc.semaphore(f"d_{j}")) for j in range(N_DMA_SEMS)]
            for e in ENGS:
                k = 0
                for o in self.ops[e]:
                    if o.is_dma:
                        o.ev = ("d", o.dsem, o.dval)
                    else:
                        if o.needs_inc:
                            k += 1
                            o.ev = ("e", e, k)
                        else:
                            o.ev = None
            self.n_waits = 0
            self.n_incs = 0

            def run(e, eng):
                waited = {}
                for o in self.ops[e]:
                    need = {}
                    for d in o.deps:
                        if not d.is_dma and d.eng == o.eng and (d.eng == "pe" or not SAME_ENG_SYNC):
                            continue
                        kind, s, val = d.ev
                        key = (kind, s)
                        if waited.get(key, 0) >= val:
                            continue
                        if need.get(key, 0) < val:
                            need[key] = val
                    for (kind, s), val in need.items():
                        sem = esems[s] if kind == "e" else dsems[s]
                        eng.wait_ge(sem, val)
                        waited[(kind, s)] = val
                        self.n_waits += 1
                    if o.is_dma:
                        for f in o.fn:
                            f(eng).then_inc(dsems[o.dsem], 16)
                    else:
                        ins = o.fn(eng)
                        if o.needs_inc:
                            ins.then_inc(esems[e], 1)
                            self.n_incs += 1
                if e == "sp":
                    for j in range(N_DMA_SEMS):
                        if self.dma_counts[j] > waited.get(("d", j), 0):
                            eng.wait_ge(dsems[j], self.dma_counts[j])

            with nc.Block() as block:
                @block.tensor
                def _(eng):
                    run("pe", eng)

                @block.scalar
                def _(eng):
                    run("act", eng)

                @block.vector
                def _(eng):
                    run("dve", eng)

                @block.gpsimd
                def _(eng):
                    run("pool", eng)

                @block.sync
                def _(eng):
                    run("sp", eng)

    def mm(self, out, lhsT, rhs, start=True, stop=True):
        return self.op("pe", lambda e: e.matmul(out.ap, lhsT.ap, rhs.ap, start=start, stop=stop),
                       reads=[lhsT, rhs] + ([] if start else [out]), writes=[out])

    def tr(self, out, in_, ident):
        return self.op("pe", lambda e: e.transpose(out.ap, in_.ap, ident.ap), reads=[in_, ident], writes=[out])

    def act(self, out, in_, func, bias=0.0, scale=1.0, accum_out=None, eng="act"):
        rd = [in_]
        b = bias.ap if isinstance(bias, V) else bias
        s = scale.ap if isinstance(scale, V) else scale
        if isinstance(bias, V):
            rd.append(bias)
        if isinstance(scale, V):
            rd.append(scale)
        wr = [out]
        kw = {}
        if accum_out is not None:
            wr.append(accum_out)
            kw["accum_out"] = accum_out.ap
        return self.op("act", lambda e: e.activation(out.ap, in_.ap, func, bias=b, scale=s, **kw), reads=rd, writes=wr)

    def ts(self, eng, out, in0, s1, op0, s2=None, op1=None, accum_out=None):
        rd = [in0]
        a1 = s1.ap if isinstance(s1, V) else s1
        a2 = s2.ap if isinstance(s2, V) else s2
        if isinstance(s1, V):
            rd.append(s1)
        if isinstance(s2, V):
            rd.append(s2)
        wr = [out]
        kw = {}
        if op1 is not None:
            kw["op1"] = op1
        if accum_out is not None:
            wr.append(accum_out)
            kw["accum_out"] = accum_out.ap
        return self.op(eng, lambda e: e.tensor_scalar(out.ap, in0.ap, a1, a2, op0, **kw), reads=rd, writes=wr)

    def tt(self, eng, out, in0, in1, op):
        return self.op(eng, lambda e: e.tensor_tensor(out.ap, in0.ap, in1.ap, op), reads=[in0, in1], writes=[out])

    def stt(self, eng, out, in0, scalar, in1, op0, op1, accum_out=None):
        rd = [in0, in1]
        a = scalar.ap if isinstance(scalar, V) else scalar
        if isinstance(scalar, V):
            rd.append(scalar)
        wr = [out]
        kw = {}
        if accum_out is not None:
            wr.append(accum_out)
            kw["accum_out"] = accum_out.ap
        return self.op(eng, lambda e: e.scalar_tensor_tensor(out.ap, in0.ap, a, in1.ap, op0, op1, **kw), reads=rd, writes=wr)

    def cp(self, eng, out, in_):
        if eng == "act":
            return self.op("act", lambda e: e.copy(out.ap, in_.ap), reads=[in_], writes=[out])
        return self.op(eng, lambda e: e.tensor_copy(out.ap, in_.ap), reads=[in_], writes=[out])

    def memset(self, eng, out, val):
        return self.op(eng, lambda e: e.memset(out.ap, val), reads=[], writes=[out])

    def load(self, eng, out, src_aps, dst_aps=None):
        if dst_aps is None:
            dst_aps = [out.ap]
        fns = [(lambda e, d=d, s=s: e.dma_start(out=d, in_=s)) for d, s in zip(dst_aps, src_aps)]
        return self.dma(eng, fns, reads=[], writes=[out])

    def store(self, eng, dst_aps, in_, src_aps=None):
        if src_aps is None:
            src_aps = [in_.ap]
        fns = [(lambda e, d=d, s=s: e.dma_start(out=d, in_=s)) for d, s in zip(dst_aps, src_aps)]
        return self.dma(eng, fns, reads=[in_], writes=[], is_output=True)


D = 1024
KC = 8
NCM = 1152
import os
PB0 = int(os.environ.get("PB0", "6"))
SB_BASE = 16512
SB_CAP = 229344
LN_EPS = 1e-5
ALPHA = 4 ** 0.25
LIMIT = 7.0
SW_ALPHA = 1.702


class Cfg:
    def __init__(self, T=2048, E=32, layers=(("mix", True), ("pool", True)), full_depth=2):
        self.T, self.E, self.layers = T, E, layers
        self.skip = set()
        self.lvl = 9
        self.dn_heads = 4
        self.ngrp = 2
        self.zf32 = False
        self.dumps = set()
        self.NT = T // 128
        self.NB = T // 512


class Alloc:
    def __init__(self, c):
        self.c = c
        self.off = SB_BASE
        self.marks = []

    def __call__(self, name, shape, dt):
        esz = 2 if dt == BF16 else 4
        n = int(np.prod(shape[1:])) * esz
        n = (n + 31) // 32 * 32
        assert self.off + n <= SB_CAP, f"SBUF overflow at {name}: {self.off + n - SB_CAP}"
        t = self.c.sb(name, shape, dt, self.off)
        self.off += n
        return t

    def mark(self):
        return self.off

    def reset(self, m):
        self.off = m


def run_interleaved(gens):
    gens = list(gens)
    while gens:
        for g in list(gens):
            try:
                next(g)
            except StopIteration:
                gens.remove(g)


def pipeline(items, nst, stage):
    for step in range(len(items) + nst - 1):
        for st in range(nst):
            i = step - st
            if 0 <= i < len(items):
                run_interleaved([stage(x, st) for x in items[i]])


def build(cfg):
    nc = bass.Bass("TRN2", target_bir_lowering=False)
    T, E, NT, NB = cfg.T, cfg.E, cfg.NT, cfg.NB
    L = len(cfg.layers)

    def din(name, shape, dt=F32):
        return nc.dram_tensor(name, list(shape), dt, kind="ExternalInput").ap()

    dr = {}
    dr["x"] = din("x", [T, D])
    dr["ident"] = din("ident", [128, 128])
    dr["ln1_g"] = din("ln1_g", [L, D])
    dr["ln1_b"] = din("ln1_b", [L, D])
    dr["ln2_g"] = din("ln2_g", [L, D])
    dr["ln2_b"] = din("ln2_b", [L, D])
    dr["router_w"] = din("router_w", [L, D, E])
    dr["router_b"] = din("router_b", [L, E])
    dr["moe_w1"] = din("moe_w1", [L, E, D, 2 * D])
    dr["moe_b1"] = din("moe_b1", [L, E, 2 * D])
    dr["moe_w2"] = din("moe_w2", [L, E, D, D])
    dr["moe_b2"] = din("moe_b2", [L, E, D])
    dr["bands"] = din("bands", [128, 12, 128])
    dr["pool_w"] = din("pool_w", [1, 4, 256, 256])
    dr["pool_b"] = din("pool_b", [1, D])
    dr["pool_scale"] = din("pool_scale", [1, D])
    dr["cmix"] = din("cmix", [128, NCM])
    dr["positions"] = din("positions", [1, T], I32)
    dr["mix_w_in"] = din("mix_w_in", [1, D, 2824])
    dr["mix_b_in"] = din("mix_b_in", [1, 2824])
    dr["dn_conv_w"] = din("dn_conv_w", [1, 4, 1536])
    dr["dn_a_log"] = din("dn_a_log", [1, 4])
    dr["dn_dt_bias"] = din("dn_dt_bias", [1, 4])
    dr["dn_norm_w"] = din("dn_norm_w", [1, 128])
    dr["swa_sinks"] = din("swa_sinks", [1, 8])
    dr["mix_w_out"] = din("mix_w_out", [1, D, D])
    dr["mix_b_out"] = din("mix_b_out", [1, D])
    out_d = nc.dram_tensor("out", [T, D], F32, kind="ExternalOutput").ap()

    c = Ctx(nc)
    A = Alloc(c)
    x_cur = A("x_cur", [128, NT, D], F32)
    xT = A("xT", [128, KC, T], BF16)
    ident = A("ident", [128, 128], F32)
    gates = A("gates", [128, NT, E], F32)
    PS = [c.ps(f"ps{i}", [128, 512], F32) for i in range(8)]

    dbg_outs = {}

    def dump(name, view, shape):
        if name not in cfg.dumps:
            return
        n = int(np.prod(shape[1:]))
        stg = c.sb("stg_" + name, [shape[0], n], F32, SB_CAP - 4 * n - 64)
        d = nc.dram_tensor("dbg_" + name, [shape[0], n], F32, kind="ExternalOutput").ap()
        c.cp("dve", stg[:, :], view)
        c.store("sp", [d[:, :]], stg[:, :])

    c.load("sp", ident[:, :], [dr["ident"][:, :]])
    xv = dr["x"].rearrange("(t p) d -> p t d", p=128)
    for tt in range(NT):
        c.load("sp", x_cur.v(tt, x_cur.h[:, tt, :]), [xv[:, tt, :]])

    def bcast_load(dst, src_row):
        c.load("sp", dst, [src_row.partition_broadcast(dst.ap.shape[0])])

    def layer_norm_all(g_bc, b_bc, small, after=None):
        st, mv, rstd = small
        NB_ = 10

        def stage(tt, sg):
            k = tt % NB_
            xt = x_cur.v(tt, x_cur.h[:, tt, :])
            stv = st.v(k, st.h[:, k * 12:(k + 1) * 12])
            mvv = mv.v(k, mv.h[:, 4 * k:4 * k + 2])
            rs = rstd.v(k, rstd.h[:, 2 * k:2 * k + 1])
            nm = rstd.v(k, rstd.h[:, 2 * k + 1:2 * k + 2])
            if sg == 0:
                for hf in range(2):
                    c.op("dve", lambda e, hf=hf: e.bn_stats(st.h[:, k * 12 + hf * 6:k * 12 + (hf + 1) * 6], x_cur.h[:, tt, hf * 512:(hf + 1) * 512]),
                         reads=[xt], writes=[stv])
                    yield
                c.op("dve", lambda e: e.bn_aggr(mv.h[:, 4 * k:4 * k + 2], st.h[:, k * 12:(k + 1) * 12]), reads=[stv], writes=[mvv])
                yield
                c.ts("dve", rs, mv.v(k, mv.h[:, 4 * k + 1:4 * k + 2]), LN_EPS, ALU.add)
                yield
            elif sg == 1:
                c.act(rs, rs, AF.Sqrt)
                yield
                c.op("dve", lambda e: e.reciprocal(rstd.h[:, 2 * k:2 * k + 1], rstd.h[:, 2 * k:2 * k + 1]), reads=[rs], writes=[rs])
                yield
                c.stt("dve", nm, mv.v(k, mv.h[:, 4 * k:4 * k + 1]), -1.0, rs, ALU.mult, ALU.mult)
                yield
            elif sg == 2:
                c.act(xt, xt, AF.Identity, bias=nm, scale=rs)
                yield
            else:
                c.tt("dve", xt, xt, g_bc[:, :], ALU.mult)
                yield
                c.tt("pool", xt, xt, b_bc[:, :], ALU.add)
                yield
                if after is not None:
                    after(tt)

        pipeline([tuple(range(t0, min(t0 + 2, NT))) for t0 in range(0, NT, 2)], 4, stage)

    def post_ln_all(l, rw32, rb_bc, b2s, bufs):
        NPB = len(bufs)

        def stage(tt, st):
            xT32, lg, sm, gTs = bufs[tt % NPB]
            m8, negm, ex, mask, ssum, rs = sm
            gt = gates.v(tt, gates.h[:, tt, :])
            if st == 0:
                for half in range(2):
                    pb = PS[(2 * tt + half) % 4]
                    for q in range(4):
                        kc = half * 4 + q
                        c.tr(pb.v(q, pb.h[:, q * 128:(q + 1) * 128]), x_cur.v(tt, x_cur.h[:, tt, kc * 128:(kc + 1) * 128]), ident[:, :])
                        yield
                    pv = V(pb, None, pb.h[:, :].rearrange("p (q n) -> p q n", q=4))
                    c.cp("act", xT.v(tt, xT.h[:, half * 4:(half + 1) * 4, tt * 128:(tt + 1) * 128]), pv)
                    yield
                    c.cp("dve", xT32.v(half, xT32.h[:, half * 4:(half + 1) * 4, :]), pv)
                    yield
            elif st == 1:
                lgp = PS[4 + tt % 2]
                for kc in range(KC):
                    c.mm(lgp.v(None, lgp.h[:, 0:E]), xT32.v(kc // 4, xT32.h[:, kc, :]), rw32.v(None, rw32.h[:, kc, :]),
                         start=(kc == 0), stop=(kc == KC - 1))
                    yield
                c.tt("dve", lg[:, :], lgp.v(None, lgp.h[:, 0:E]), rb_bc[:, :], ALU.add)
                yield
                c.op("dve", lambda e: e.max(m8.h[:, :], lg.h[:, :]), reads=[lg[:, :]], writes=[m8[:, :]])
                yield
                c.ts("dve", negm[:, :], m8.v(None, m8.h[:, 0:1]), -1.0, ALU.mult)
                yield
            elif st == 2:
                c.act(ex[:, :], lg[:, :], AF.Exp, bias=negm[:, 0:1], scale=1.0)
                yield
                c.ts("dve", mask[:, :], lg[:, :], m8.v(None, m8.h[:, 3:4]), ALU.is_ge)
                yield
                c.tt("dve", ex[:, :], ex[:, :], mask[:, :], ALU.mult)
                yield
                c.op("dve", lambda e: e.reduce_sum(ssum.h[:, :], ex.h[:, :], AX.X), reads=[ex[:, :]], writes=[ssum[:, :]])
                yield
                c.op("dve", lambda e: e.reciprocal(rs.h[:, :], ssum.h[:, :]), reads=[ssum[:, :]], writes=[rs[:, :]])
                yield
                c.ts("dve", gt, ex[:, :], rs[:, 0:1], ALU.mult)
                yield
            else:
                gp = PS[6 + tt % 2]
                c.tr(gp.v(None, gp.h[0:E, 0:128]), gt, ident[:, :])
                yield
                c.cp("act", gTs[:, :], gp.v(None, gp.h[0:E, 0:128]))
                yield
                for half in range(2):
                    pb = PS[(2 * tt + half) % 4]
                    c.mm(pb[:, :], gTs[:, :], b2s.v(None, b2s.h[:, half * 512:(half + 1) * 512]))
                    yield
                    xh = x_cur.v(tt, x_cur.h[:, tt, half * 512:(half + 1) * 512])
                    c.stt("dve", xh, xh, ALPHA, pb[:, :], ALU.mult, ALU.add)
                    yield

        pipeline([tuple(range(t0, min(t0 + 2, NT))) for t0 in range(0, NT, 2)], 4, stage)

    NBUF = 3

    def moe_load_unit(l, u, w1u, w2u):
        e, q = u // 4, u % 4
        b = u % NBUF
        src1 = dr["moe_w1"][l, e].rearrange("(kc p) n -> p kc n", p=128)
        c.load("pool", w1u[b][:, :, :], [src1[:, :, 512 * q:512 * q + 512]])
        src2 = dr["moe_w2"][l, e, 256 * q:256 * q + 256, :].rearrange("(j p) n -> p j n", p=128)
        c.load("pool", w2u[b][:, :, :], [src2])

    def moe(l, m0, w1u, w2u):
        A.reset(m0)
        CAP = SW_ALPHA * LIMIT / (1.0 + float(np.exp(-SW_ALPHA * LIMIT)))
        actT = [A(f"actT{i}", [128, 2, T], BF16) for i in range(2)]
        NTMP = 3
        tsg = [A(f"tsg{i}", [128, 512], F32) for i in range(NTMP)]
        tlr = [A(f"tlr{i}", [128, 512], F32) for i in range(NTMP)]
        b1s = A("b1s", [E, 2 * D], F32)
        b1T = A("b1T", [128, 16, E], F32)
        c.load("sp", b1s[:, :], [dr["moe_b1"][l, :, :]])
        pb = PS[6]
        for cc in range(16):
            j, gl = cc // 2, cc % 2
            c.tr(pb.v(cc, pb.h[:, cc * E:(cc + 1) * E]), b1s.v(None, b1s.h[:, 256 * j + gl:256 * j + 256:2]),
                 ident.v(None, ident.h[0:E, 0:E]))
        c.cp("dve", b1T[:, :, :], V(pb, None, pb.h[:, 0:16 * E].rearrange("p (c e) -> p c e", c=16)))
        bgv = b1T.v(None, b1T.h[:, 0:16:2, :])
        blv = b1T.v(None, b1T.h[:, 1:16:2, :])
        c.ts("dve", bgv, bgv, SW_ALPHA, ALU.mult)
        c.ts("dve", blv, blv, 1.0, ALU.add, 1.0 / SW_ALPHA, ALU.mult)

        units = [(e, q) for e in range(E) for q in range(4)]
        w1v = dr["moe_w1"]
        w2v = dr["moe_w2"]

        def load_unit(u):
            moe_load_unit(l, u, w1u, w2u)

        cnt = [0]
        ycnt = [0]
        pendF = []

        def h_group(u, tb, j2, gl):
            e, q = units[u]
            b = u % NBUF
            j = 2 * q + j2
            i = cnt[0] % NTMP
            hp = PS[(cnt[0] % 2) + 2 * gl]
            for kc in range(KC):
                c.mm(hp[:, :], w1u[b].v(None, w1u[b].h[:, kc, 256 * j2 + gl:256 * j2 + 256:2]),
                     xT.v(None, xT.h[:, kc, tb * 512:(tb + 1) * 512]), start=(kc == 0), stop=(kc == KC - 1))
            if gl == 0:
                c.act(tsg[i][:, :], hp[:, :], AF.Silu, bias=b1T.v(None, b1T.h[:, 2 * j, e:e + 1]), scale=SW_ALPHA)
            else:
                c.act(tlr[i][:, :], hp[:, :], AF.Identity, bias=b1T.v(None, b1T.h[:, 2 * j + 1, e:e + 1]), scale=1.0 / SW_ALPHA)
                c.ts("pool", tlr[i][:, :], tlr[i][:, :], (LIMIT + 1.0) / SW_ALPHA, ALU.min, (1.0 - LIMIT) / SW_ALPHA, ALU.max)
                ab = u % 2
                dst = actT[ab].v((j2, tb), actT[ab].h[:, j2, tb * 512:(tb + 1) * 512])
                pendF.append((dst, tsg[i], tlr[i]))
                cnt[0] += 1

        def flushF(keep):
            while len(pendF) > keep:
                dst, a_, b_ = pendF.pop(0)
                c.stt("dve", dst, a_[:, :], CAP, b_[:, :], ALU.min, ALU.mult)

        def y_groups(u, tb, gs):
            e, q = units[u]
            b = u % NBUF
            ab = u % 2
            for g in gs:
                tt = tb * 4 + g // 2
                half = g % 2
                yp = PS[4 + ycnt[0] % 4]
                ycnt[0] += 1
                for j2 in range(2):
                    c.mm(yp[:, :], actT[ab].v((j2, tb), actT[ab].h[:, j2, tt * 128:(tt + 1) * 128]),
                         w2u[b].v(None, w2u[b].h[:, j2, half * 512:(half + 1) * 512]), start=(j2 == 0), stop=(j2 == 1))
                xh = x_cur.v(tt, x_cur.h[:, tt, half * 512:(half + 1) * 512])
                c.stt("dve", xh, yp[:, :], gates.v(tt, gates.h[:, tt, e:e + 1]), xh, ALU.mult, ALU.add)

        NU = len(units)
        for u in range(NU + 1):
            for tb in range(NB):
                k = 0
                for j2 in range(2):
                    for gl in range(2):
                        if u < NU:
                            h_group(u, tb, j2, gl)
                        if u > 0:
                            y_groups(u - 1, tb, (2 * k, 2 * k + 1))
                        k += 1
                        if k == 2:
                            flushF(1 if u < NU else 0)
                        if k == 4:
                            flushF(1 if (u < NU and tb < NB - 1) else 0)
            if u + 2 < NU:
                load_unit(u + 2)
        flushF(0)

    def pool_mixer(i):
        bands = A("bands", [128, 12, 128], F32)
        pw = A("pw", [128, 4, 2, 256], BF16)
        pbb = A("pbb", [128, D], F32)
        psb = A("psb", [128, D], F32)
        pooledT = A("pooledT", [128, KC, T], BF16)
        tmp = [A(f"ptmp{k}", [128, 512], F32) for k in range(2)]
        c.load("sp", bands[:, :, :], [dr["bands"][:, :, :]])
        for g in range(4):
            c.load("pool", pw.v(g, pw.h[:, g, :, :]), [dr["pool_w"][i, g].rearrange("(kc p) n -> p kc n", p=128)])
        bcast_load(pbb[:, :], dr["pool_b"][i:i + 1, :])
        bcast_load(psb[:, :], dr["pool_scale"][i:i + 1, :])
        k = 0
        for n in range(NT):
            for half in range(2):
                pb = PS[k % 2]
                k += 1
                for q in range(4):
                    cc = half * 4 + q
                    g = cc // 2
                    o = pb.v(None, pb.h[:, q * 128:(q + 1) * 128])
                    cur = x_cur.v(n, x_cur.h[:, n, cc * 128:(cc + 1) * 128])
                    if n == 0:
                        c.mm(o, cur, bands.v(None, bands.h[:, 3 * g + 0, :]))
                    else:
                        prev = x_cur.v(n - 1, x_cur.h[:, n - 1, cc * 128:(cc + 1) * 128])
                        c.mm(o, prev, bands.v(None, bands.h[:, 3 * g + 2, :]), start=True, stop=False)
                        c.mm(o, cur, bands.v(None, bands.h[:, 3 * g + 1, :]), start=False, stop=True)
                c.cp("act" if half == 0 else "dve", pooledT.v(n, pooledT.h[:, half * 4:(half + 1) * 4, n * 128:(n + 1) * 128]),
                     V(pb, None, pb.h[:, :].rearrange("p (q n) -> p q n", q=4)))
        tmp4 = tmp + [A(f"ptmpx{k2}", [128, 512], F32) for k2 in range(2)]

        def pool_out(item, st_):
            n, half = item
            idx_ = 2 * n + half
            pb = PS[2 + idx_ % 4]
            tm = tmp4[idx_ % 4]
            for gg in range(2):
                g = half * 2 + gg
                for kc in range(2):
                    c.mm(pb.v(None, pb.h[:, gg * 256:(gg + 1) * 256]), pooledT.v(n, pooledT.h[:, 2 * g + kc, n * 128:(n + 1) * 128]),
                         pw.v(g, pw.h[:, g, kc, :]), start=(kc == 0), stop=(kc == 1))
            yield
            sl = slice(half * 512, (half + 1) * 512)
            c.tt("dve", tm[:, :], pb[:, :], pbb.v(None, pbb.h[:, sl]), ALU.add)
            yield
            c.tt("dve", tm[:, :], tm[:, :], psb.v(None, psb.h[:, sl]), ALU.mult)
            yield
            xh = x_cur.v(n, x_cur.h[:, n, sl])
            c.stt("dve", xh, xh, ALPHA, tm[:, :], ALU.mult, ALU.add)
            yield

        pipeline([((n, 0), (n, 1)) for n in range(NT)], 1, pool_out)

    def mixer0(i):
        PI = float(np.pi)
        psrr = [0]

        def nb():
            b = PS[psrr[0] % 8]
            psrr[0] += 1
            return b

        b_in = dr["mix_b_in"]
        w_in = dr["mix_w_in"][i].rearrange("(kc p) n -> p kc n", p=128)
        w_outv = dr["mix_w_out"][i]
        cm = A("cm", [128, NCM], F32)
        c.load("sp", cm[:, :], [dr["cmix"][:, :]])
        Uc = cm.v(None, cm.h[:, 0:128])
        ones = cm.v(None, cm.h[:, 128:256])
        Prot = cm.v(None, cm.h[:, 256:384])
        mlow = cm.v(None, cm.h[:, 384:512])
        mupi = cm.v(None, cm.h[:, 512:640])
        swam = cm.h[:, 640:896]
        freq = cm.v(None, cm.h[:, 896:897])
        id32 = cm.v(None, cm.h[:, 897:1025])
        identb = A("identb", [128, 128], BF16)
        c.cp("dve", identb[:, :], id32)
        binT = A("binT", [128, 22], F32)
        bkd = A("bkd", [128, 2], F32)
        for kv in range(2):
            src = b_in[i, 512 + 64 * kv:576 + 64 * kv].rearrange("(p o) -> p o", o=1)
            c.load("sp", bkd.v(None, bkd.h[0:64, kv:kv + 1]), [src])
            c.load("sp", bkd.v(None, bkd.h[64:128, kv:kv + 1]), [src])
        bv_bc = A("bv_bc", [128, 128], F32)
        bab_bc = A("bab_bc", [128, 8], F32)
        alog_bc = A("alog_bc", [128, 4], F32)
        dtb_bc = A("dtb_bc", [128, 4], F32)
        nw_bc = A("nw_bc", [128, 128], F32)
        sink_bc = A("sink_bc", [128, 8], F32)
        bcast_load(bv_bc[:, :], b_in[i:i + 1, 640:768])
        bcast_load(bab_bc[:, :], b_in[i:i + 1, 2816:2824])
        bcast_load(alog_bc[:, :], dr["dn_a_log"][i:i + 1, :])
        bcast_load(dtb_bc[:, :], dr["dn_dt_bias"][i:i + 1, :])
        bcast_load(nw_bc[:, :], dr["dn_norm_w"][i:i + 1, :])
        bcast_load(sink_bc[:, :], dr["swa_sinks"][i:i + 1, :])
        cwT = A("cwT", [128, 12, 4], F32)
        m_small = A.mark()
        bout_bc = A("bout_bc", [128, D], F32)
        bcast_load(bout_bc[:, :], dr["mix_b_out"][i:i + 1, :])
        bin22 = A("bin22", [22, 128], F32)
        c.load("sp", bin22[:, :], [b_in[i, 0:2816].rearrange("(c p) -> c p", p=128)])
        pb = nb()
        c.tr(pb.v(None, pb.h[:, 0:22]), bin22[:, :], id32.t.v(None, cm.h[0:22, 897:897 + 22]))
        c.cp("dve", binT[:, :], pb.v(None, pb.h[:, 0:22]))
        cw4 = A("cw4", [4, 1536], F32)
        c.load("sp", cw4[:, :], [dr["dn_conv_w"][i, :, :]])
        pb = nb()
        for cc in range(12):
            c.tr(pb.v(None, pb.h[:, cc * 4:(cc + 1) * 4]), cw4.v(None, cw4.h[:, cc * 128:(cc + 1) * 128]),
                 id32.t.v(None, cm.h[0:4, 897:901]))
        c.cp("dve", cwT[:, :, :], V(pb, None, pb.h[:, 0:48].rearrange("p (c j) -> p c j", c=12)))
        for tt in range(NT):
            xt = x_cur.v(tt, x_cur.h[:, tt, :])
            c.stt("dve", xt, xt, ALPHA, bout_bc[:, :], ALU.mult, ALU.add)
        A.reset(m_small)

        if "swa" not in cfg.skip:
            w_q = A("w_q", [128, KC, 512], BF16)
            w_kd = A("w_kd", [128, KC, 2, 128], BF16)
            w_v = A("w_v", [128, KC, 128], BF16)
            w_oA = A("w_oA", [128, 4, D], BF16)
            c.load("pool", w_q[:, :, :], [w_in[:, :, 0:512]])
            for kv in range(2):
                for rep in range(2):
                    c.load("pool", w_kd.v(None, w_kd.h[:, :, kv, rep * 64:(rep + 1) * 64]), [w_in[:, :, 512 + 64 * kv:576 + 64 * kv]])
            c.load("pool", w_v[:, :, :], [w_in[:, :, 640:768]])
            c.load("pool", w_oA[:, :, :], [w_outv[0:512, :].rearrange("(kc p) n -> p kc n", p=128)])
            qT = A("qT", [128, 4, T], BF16)
            kTd = A("kTd", [128, 2, T], BF16)
            vtok = A("vtok", [128, NT, 2, 192], BF16)
            c.memset("pool", vtok[:, :, :, :], 0.0)
            aoT = A("aoT", [128, 4, T], BF16)
            m_rot = A.mark()
            posi = A("posi", [128, 512], I32)
            ang = A("ang", [128, 512], F32)
            Sn = A("Sn", [128, 512], F32)
            Cn = A("Cn", [128, 512], F32)
            qtmp = [A(f"qtmp{k}", [128, 512], F32) for k in range(2)]
            t1 = [A(f"t1{k}", [128, 512], F32) for k in range(2)]
            uu = [A(f"uu{k}", [128, 512], F32) for k in range(2)]
            kf = A("kf", [128, 512], F32)
            ki = A("ki", [128, 512], I32)
            rk = [0]

            def rot_block(ps_src, bias_v, SC, dst):
                k = rk[0] % 2
                rk[0] += 1
                c.act(qtmp[k][:, :], ps_src[:, :], AF.Identity, bias=bias_v)
                pw_ = nb()
                c.mm(pw_[:, :], Prot, qtmp[k][:, :])
                c.tt("dve", t1[k][:, :], qtmp[k][:, :], Cn[:, :], ALU.mult)
                c.stt("dve", uu[k][:, :], pw_[:, :], SC, Sn[:, :], ALU.mult, ALU.mult)
                c.stt("dve", dst, t1[k][:, :], SC, uu[k][:, :], ALU.mult, ALU.add)

            for tb in range(NB):
                bs = slice(tb * 512, (tb + 1) * 512)
                c.load("sp", posi[:, :], [dr["positions"][0:1, bs].partition_broadcast(128)])
                c.cp("dve", ang[:, :], posi[:, :])
                c.ts("dve", ang[:, :], ang[:, :], freq, ALU.mult)
                for tab, shift in ((Sn, 0.0), (Cn, PI / 2)):
                    c.ts("dve", tab[:, :], ang[:, :], shift, ALU.add)
                    c.ts("dve", kf[:, :], tab[:, :], 1.0 / (2 * PI), ALU.mult)
                    c.cp("dve", ki[:, :], kf[:, :])
                    c.cp("dve", kf[:, :], ki[:, :])
                    c.stt("dve", tab[:, :], kf[:, :], -6.28125, tab[:, :], ALU.mult, ALU.add)
                    c.stt("dve", tab[:, :], kf[:, :], -(2 * PI - 6.28125), tab[:, :], ALU.mult, ALU.add)
                    c.ts("dve", tab[:, :], tab[:, :], PI, ALU.min, -PI, ALU.max)
                    c.act(tab[:, :], tab[:, :], AF.Sin)
                for ch in range(4):
                    pq = nb()
                    for kc in range(KC):
                        c.mm(pq[:, :], w_q.v(None, w_q.h[:, kc, ch * 128:(ch + 1) * 128]), xT.v(None, xT.h[:, kc, bs]),
                             start=(kc == 0), stop=(kc == KC - 1))
                    rot_block(pq, binT.v(None, binT.h[:, ch:ch + 1]), 0.125, qT.v((ch, tb), qT.h[:, ch, bs]))
                for kv in range(2):
                    pk = nb()
                    for kc in range(KC):
                        c.mm(pk[:, :], w_kd.v(None, w_kd.h[:, kc, kv, :]), xT.v(None, xT.h[:, kc, bs]),
                             start=(kc == 0), stop=(kc == KC - 1))
                    rot_block(pk, bkd.v(None, bkd.h[:, kv:kv + 1]), 1.0, kTd.v((kv, tb), kTd.h[:, kv, bs]))
            for tt in range(NT):
                pv_ = nb()
                for kc in range(KC):
                    c.mm(pv_.v(None, pv_.h[:, 0:128]), xT.v(None, xT.h[:, kc, tt * 128:(tt + 1) * 128]), w_v.v(None, w_v.h[:, kc, :]),
                         start=(kc == 0), stop=(kc == KC - 1))
                c.tt("dve", vtok.v(None, vtok.h[:, tt, :, 64:128]), V(pv_, None, pv_.h[:, 0:128].rearrange("p (a b) -> p a b", a=2)),
                     V(bv_bc, None, bv_bc.h[:, :].rearrange("p (a b) -> p a b", a=2)), ALU.add)
            dump("qT", qT.v(None, qT.h[:, 0, 0:512]), [128, 512])
            dump("kT", kTd.v(None, kTd.h[:, 0, 0:512]), [128, 512])
            dump("Sn", Sn[:, :], [128, 512])
            dump("Cn", Cn[:, :], [128, 512])
            dump("vtok", vtok.v(None, vtok.h[:, 0, 0, :]), [128, 192])
            dump("x0", x_cur.v(None, x_cur.h[:, 0, 0:512]), [128, 512])
            A.reset(m_rot)
            NS = 9
            smb = [A(f"smb{k}", [128, 256], F32) for k in range(NS)]
            pex = [A(f"pex{k}", [128, 256], F32) for k in range(NS)]
            pnb = [A(f"pnb{k}", [128, 256], BF16) for k in range(NS)]
            pTs = [A(f"pTs{k}", [128, 2, 128], BF16) for k in range(NS)]
            sc1 = [A(f"sc1{k}", [128, 8], F32) for k in range(NS)]
            its = [(n, ch, hh) for n in range(NT) for ch in range(4) for hh in range(2)]
            po_of = {}

            def att_stage(idx, st):
                n, ch, hh = its[idx]
                k = idx % NS
                kv = ch // 2
                h = 2 * ch + hh
                k0 = 0 if n > 0 else 128
                NK = 256 - k0
                nkb = NK // 128
                keys = slice(n * 128 - 128 + k0, (n + 1) * 128)
                rows = slice(64 * hh, 64 * hh + 64)
                s1 = sc1[k]
                col = lambda a_: s1.v(None, s1.h[:, a_:a_ + 1])
                sm_ = smb[k].v(None, smb[k].h[:, 0:NK])
                pe_ = pex[k].v(None, pex[k].h[:, 0:NK])
                pn_ = pnb[k].v(None, pnb[k].h[:, 0:NK])
                if st == 0:
                    pa = nb()
                    c.mm(pa.v(None, pa.h[:, 0:NK]), qT.v((ch, n // 4), qT.h[rows, ch, n * 128:(n + 1) * 128]),
                         kTd.v(None, kTd.h[rows, kv, keys]))
                    yield
                    c.tt("dve", sm_, pa.v(None, pa.h[:, 0:NK]), cm.v(None, swam[:, k0:256]), ALU.add)
                    yield
                    c.op("dve", lambda e: e.reduce_max(s1.h[:, 0:1], smb[k].h[:, 0:NK], AX.X), reads=[sm_], writes=[col(0)])
                    yield
                    c.tt("dve", col(1), col(0), sink_bc.v(None, sink_bc.h[:, h:h + 1]), ALU.max)
                    yield
                    c.ts("dve", col(2), col(1), -1.0, ALU.mult)
                    yield
                elif st == 1:
                    c.act(pe_, sm_, AF.Exp, bias=col(2), accum_out=col(3))
                    yield
                    c.act(col(4), col(2), AF.Exp, bias=sink_bc.v(None, sink_bc.h[:, h:h + 1]))
                    yield
                    c.tt("dve", col(5), col(3), col(4), ALU.add)
                    yield
                    c.op("dve", lambda e: e.reciprocal(s1.h[:, 6:7], s1.h[:, 5:6]), reads=[col(5)], writes=[col(6)])
                    yield
                    c.act(pn_, pe_, AF.Copy, scale=col(6))
                    yield
                elif st == 2:
                    ptp = nb()
                    ptb = ptp.h[:, :].bitcast(BF16)
                    for kb in range(nkb):
                        c.tr(V(ptp, None, ptb[:, kb * 128:(kb + 1) * 128]), pnb[k].v(None, pnb[k].h[:, kb * 128:(kb + 1) * 128]), identb[:, :])
                        yield
                    c.cp("act", pTs[k].v(None, pTs[k].h[:, 0:nkb, :]), V(ptp, None, ptb[:, 0:nkb * 128].rearrange("p (a b) -> p a b", a=nkb)))
                    yield
                else:
                    if hh == 0:
                        po_of[(n, ch)] = nb()
                    po = po_of[(n, ch)]
                    for kb in range(nkb):
                        ktile = n - (nkb - 1) + kb
                        vsl = slice(64, 192) if hh == 0 else slice(0, 128)
                        c.mm(po.v(None, po.h[:, 0:128]), vtok.v(None, vtok.h[:, ktile, kv, vsl]),
                             pTs[k].v(None, pTs[k].h[:, kb, :]), start=(hh == 0 and kb == 0), stop=(hh == 1 and kb == nkb - 1))
                        yield
                    if hh == 1:
                        c.cp("dve", aoT.v((ch, n), aoT.h[:, ch, n * 128:(n + 1) * 128]), po.v(None, po.h[:, 0:128]))
                        yield

            pipeline([(i2, i2 + 1) for i2 in range(0, len(its), 2)], 4, att_stage)
            dump("aoT", aoT.v(None, aoT.h[:, 0, 0:512]), [128, 512])
            for tt in range(NT):
                for half in range(2):
                    py = nb()
                    for ch in range(4):
                        c.mm(py[:, :], aoT.v((ch, tt), aoT.h[:, ch, tt * 128:(tt + 1) * 128]),
                             w_oA.v(None, w_oA.h[:, ch, half * 512:(half + 1) * 512]), start=(ch == 0), stop=(ch == 3))
                    xh = x_cur.v(tt, x_cur.h[:, tt, half * 512:(half + 1) * 512])
                    c.tt("dve", xh, xh, py[:, :], ALU.add)

        if "dn" in cfg.skip:
            return
        A.reset(m_small)
        bz_bc = A("bz_bc", [128, 512], F32)
        bcast_load(bz_bc[:, :], b_in[i:i + 1, 2304:2816])
        madd_low = A("madd_low", [128, 512], F32)
        madd_upi = A("madd_upi", [128, 512], F32)
        id4 = A("id4", [128, 512], F32)
        for r4 in range(4):
            sl = slice(r4 * 128, (r4 + 1) * 128)
            c.ts("dve", madd_low.v(None, madd_low.h[:, sl]), mlow, 30000.0, ALU.mult, -30000.0, ALU.add)
            c.ts("dve", madd_upi.v(None, madd_upi.h[:, sl]), mupi, 30000.0, ALU.mult, -30000.0, ALU.add)
            c.cp("dve", id4.v(None, id4.h[:, sl]), id32)
        w_ab = A("w_ab", [128, KC, 8], BF16)
        c.load("pool", w_ab[:, :, :], [w_in[:, :, 2816:2824]])
        nea = A("nea", [128, 4], F32)
        c.act(nea[:, :], alog_bc[:, :], AF.Exp)
        c.ts("dve", nea[:, :], nea[:, :], -1.0, ALU.mult)
        tabs = {nm: A(nm, [128, NT, 4], F32) for nm in ("gcs", "ngc", "eg", "egl", "ekd", "beta", "nbeta", "nebg")}
        aball = A("aball", [128, NT, 8], F32)
        gtmp = [A(f"gtmp{k}", [128, NT, 4], F32) for k in range(4)]
        glall = A("glall", [128, NT, 4], F32)
        NTB = NT // 4 if NT >= 4 else 1
        for g4 in range(0, NT, 4):
            pab = nb()
            for q4 in range(min(4, NT - g4)):
                tt = g4 + q4
                for kc in range(KC):
                    c.mm(pab.v(None, pab.h[:, q4 * 8:(q4 + 1) * 8]), xT.v(None, xT.h[:, kc, tt * 128:(tt + 1) * 128]), w_ab.v(None, w_ab.h[:, kc, :]),
                         start=(kc == 0), stop=(kc == KC - 1))
            n4 = min(4, NT - g4)
            for q4 in range(n4):
                c.tt("dve", aball.v(None, aball.h[:, g4 + q4, :]), pab.v(None, pab.h[:, q4 * 8:(q4 + 1) * 8]), bab_bc[:, :], ALU.add)
        T4 = lambda nm: tabs[nm][:, :, :]
        da_v = aball.v(None, aball.h[:, :, 0:4])
        db_v = aball.v(None, aball.h[:, :, 4:8])
        c.act(gtmp[0][:, :, :], db_v, AF.Exp, scale=-1.0)
        c.ts("dve", gtmp[0][:, :, :], gtmp[0][:, :, :], 1.0, ALU.add)
        c.op("dve", lambda e: e.reciprocal(tabs["beta"].h[:, :, :], gtmp[0].h[:, :, :]), reads=[gtmp[0][:, :, :]], writes=[T4("beta")])
        c.ts("dve", T4("nbeta"), T4("beta"), -1.0, ALU.mult)
        for tt in range(NT):
            c.tt("dve", gtmp[1].v(None, gtmp[1].h[:, tt, :]), aball.v(None, aball.h[:, tt, 0:4]), dtb_bc[:, :], ALU.add)
        c.ts("dve", gtmp[2][:, :, :], gtmp[1][:, :, :], -1.0, ALU.mult)
        c.tt("dve", gtmp[2][:, :, :], gtmp[2][:, :, :], gtmp[1][:, :, :], ALU.max)
        c.act(gtmp[2][:, :, :], gtmp[2][:, :, :], AF.Exp, scale=-1.0)
        c.act(gtmp[2][:, :, :], gtmp[2][:, :, :], AF.Ln, bias=1.0)
        c.stt("dve", gtmp[3][:, :, :], gtmp[1][:, :, :], 0.0, gtmp[2][:, :, :], ALU.max, ALU.add)
        for tt in range(NT):
            c.tt("dve", gtmp[3].v(None, gtmp[3].h[:, tt, :]), gtmp[3].v(None, gtmp[3].h[:, tt, :]), nea[:, :], ALU.mult)
        pg = nb()
        for tt in range(NT):
            c.mm(pg.v(None, pg.h[:, tt * 8:tt * 8 + 4]), Uc, gtmp[3].v(None, gtmp[3].h[:, tt, :]))
            c.mm(pg.v(None, pg.h[:, tt * 8 + 4:tt * 8 + 8]), ones, gtmp[3].v(None, gtmp[3].h[:, tt, :]))
        pgv = pg.h[:, 0:NT * 8].rearrange("p (t e) -> p t e", e=8)
        c.cp("dve", T4("gcs"), V(pg, None, pgv[:, :, 0:4]))
        c.cp("dve", glall[:, :, :], V(pg, None, pgv[:, :, 4:8]))
        c.ts("dve", T4("ngc"), T4("gcs"), -1.0, ALU.mult)
        c.act(T4("eg"), T4("gcs"), AF.Exp)
        c.act(T4("egl"), glall[:, :, :], AF.Exp)
        c.tt("dve", gtmp[0][:, :, :], glall[:, :, :], T4("gcs"), ALU.subtract)
        c.act(T4("ekd"), gtmp[0][:, :, :], AF.Exp)
        c.stt("dve", T4("nebg"), T4("beta"), -1.0, T4("eg"), ALU.mult, ALU.mult)
        m_dn = A.mark()

        def tcol(nm, tt, h):
            return tabs[nm].v(tt, tabs[nm].h[:, tt, h:h + 1])

        for h in range(4):
            if h >= cfg.dn_heads:
                break
            A.reset(m_dn)
            w_h = A("w_h", [128, KC, 3, 128], BF16)
            w_z = A("w_z", [128, KC, 128], BF16)
            w_oB = A("w_oB", [128, D], BF16)
            for ct in range(3):
                c0 = 768 + 512 * ct + 128 * h
                c.load("pool", w_h.v(ct, w_h.h[:, :, ct, :]), [w_in[:, :, c0:c0 + 128]])
            c.load("pool", w_z[:, :, :], [w_in[:, :, 2304 + 128 * h:2432 + 128 * h]])
            c.load("pool", w_oB[:, :], [w_outv[512 + 128 * h:640 + 128 * h, :]])
            qTn = A("qTn", [128, T], F32)
            kTn = A("kTn", [128, T], F32)
            kdec = A("kdec", [128, NT, 128], F32)
            vb = A("vb", [128, NT, 128], F32)
            TT = A("TT", [128, T], F32)
            QKT = A("QKT", [128, T], F32)
            Sst = A("Sst", [128, 128], F32)
            dnT2 = [A(f"dnT{k}", [128, 128], BF16) for k in range(2)]
            zall = A("zall", [128, NT, 128], F32 if cfg.zf32 else BF16)
            zb = [A(f"zb{k}", [128, 128], F32) for k in range(2)]
            for tt in range(NT):
                pz = nb()
                for kc in range(KC):
                    c.mm(pz.v(None, pz.h[:, 0:128]), xT.v(None, xT.h[:, kc, tt * 128:(tt + 1) * 128]), w_z.v(None, w_z.h[:, kc, :]),
                         start=(kc == 0), stop=(kc == KC - 1))
                c.tt("dve", zb[tt % 2][:, :], pz.v(None, pz.h[:, 0:128]), bz_bc.v(None, bz_bc.h[:, 128 * h:128 * h + 128]), ALU.add)
                c.act(zall.v(tt, zall.h[:, tt, :]), zb[tt % 2][:, :], AF.Silu)
            smalls = [A(f"dsm{k}", [128, 4], F32) for k in range(2)]
            tok = [A(f"tok{k}", [128, 128], F32) for k in range(2)]
            junk = A("junk", [128, 128], F32)
            m_conv = A.mark()
            pre = A("pre", [128, T + 4], F32)
            cbuf = A("cbuf", [128, T], F32)
            sqb = [A(f"sqb{k}", [128, 512], F32) for k in range(4)]
            c.memset("dve", pre.v("pad", pre.h[:, 0:3]), 0.0)
            kk_ = [0]
            pend_eps = []
            for ct in range(3):
                cidx = ct * 4 + h
                for tb in range(NB):
                    bs = slice(tb * 512, (tb + 1) * 512)
                    pp = nb()
                    for kc in range(KC):
                        c.mm(pp[:, :], w_h.v(ct, w_h.h[:, kc, ct, :]), xT.v(None, xT.h[:, kc, bs]), start=(kc == 0), stop=(kc == KC - 1))
                    c.act(pre.v(tb, pre.h[:, 3 + tb * 512:3 + (tb + 1) * 512]), pp[:, :], AF.Identity, bias=binT.v(None, binT.h[:, 6 + cidx:7 + cidx]))
                for tb in range(NB):
                    cb = cbuf.v(tb, cbuf.h[:, tb * 512:(tb + 1) * 512])
                    prev_dep = [pre.v(tb - 1, pre.h[:, 0:1])] if tb > 0 else [pre.v("pad", pre.h[:, 0:1])]
                    w3 = cwT.v(None, cwT.h[:, cidx, 3:4])
                    c.op("dve", lambda e, tb=tb, w3=w3: e.tensor_scalar(cbuf.h[:, tb * 512:(tb + 1) * 512], pre.h[:, 3 + tb * 512:3 + (tb + 1) * 512], w3.ap, None, ALU.mult),
                         reads=[pre.v(tb, pre.h[:, 0:1]), w3], writes=[cb])
                    for j in (2, 1, 0):
                        wj = cwT.v(None, cwT.h[:, cidx, j:j + 1])
                        c.op("dve", lambda e, tb=tb, j=j, wj=wj: e.scalar_tensor_tensor(cbuf.h[:, tb * 512:(tb + 1) * 512], pre.h[:, j + tb * 512:j + (tb + 1) * 512],
                                                                                      wj.ap, cbuf.h[:, tb * 512:(tb + 1) * 512], ALU.mult, ALU.add),
                             reads=[pre.v(tb, pre.h[:, 0:1]), wj, cb] + prev_dep, writes=[cb])
                    c.act(cb, cb, AF.Silu)
                    if ct < 2:
                        sq = sqb[tb % 4]
                        c.act(sq[:, :], cb, AF.Square)
                        pss = nb()
                        c.mm(pss[:, :], ones, sq[:, :])
                        if pend_eps:
                            sq_, pss_ = pend_eps.pop()
                            c.ts("dve", sq_[:, :], pss_[:, :], 1e-6, ALU.add)
                        pend_eps.append((sq, pss))
                if ct < 2:
                    sq_, pss_ = pend_eps.pop()
                    c.ts("dve", sq_[:, :], pss_[:, :], 1e-6, ALU.add)
                    for tb in range(NB):
                        sq = sqb[tb % 4]
                        c.act(sq[:, :], sq[:, :], AF.Sqrt)
                        c.op("dve", lambda e, sq=sq: e.reciprocal(sq.h[:, :], sq.h[:, :]), reads=[sq[:, :]], writes=[sq[:, :]])
                        dstT = qTn if ct == 0 else kTn
                        for t4 in range(4):
                            tt = tb * 4 + t4
                            c.stt("dve", dstT.v(tt, dstT.h[:, tt * 128:(tt + 1) * 128]), V(cbuf, tb, cbuf.h[:, tt * 128:(tt + 1) * 128]),
                                  (128.0 ** -0.5) if ct == 0 else 1.0, sq.v(None, sq.h[:, t4 * 128:(t4 + 1) * 128]), ALU.mult, ALU.mult)
                if ct == 0:
                    continue
                for tt in range(NT):
                    ts_ = slice(tt * 128, (tt + 1) * 128)
                    ptk = nb()
                    pt_ = ptk.v(None, ptk.h[:, 0:128])
                    if ct == 2:
                        c.tr(pt_, cbuf.v(tt // 4, cbuf.h[:, ts_]), id32)
                        c.ts("dve", vb.v(tt, vb.h[:, tt, :]), pt_, tcol("beta", tt, h), ALU.mult)
                    else:
                        c.tr(pt_, kTn.v(tt, kTn.h[:, ts_]), id32)
                        c.act(kdec.v(tt, kdec.h[:, tt, :]), pt_, AF.Copy, scale=tcol("ekd", tt, h))
            A.reset(m_conv)
            NG = NT // 4
            NGRP = cfg.ngrp
            for g0 in range(0, NG, NGRP):
                A.reset(m_conv)
                grp = [g for g in range(g0, g0 + NGRP) if g < NG]
                Pb = {g: [A(f"P{g % NGRP}_{k}", [128, 512], F32) for k in range(2)] for g in grp}
                PTb = {g: [A(f"PT{g % NGRP}_{k}", [128, 512], F32) for k in range(2)] for g in grp}
                dgb = {g: Pb[g][1] for g in grp}
                dcl = {g: PTb[g][1] for g in grp}
                dcu = {g: A(f"dcu{g % NGRP}", [128, 512], F32) for g in grp}
                def prep(g):
                    gs = slice(g * 512, (g + 1) * 512)
                    pkk, pqk, pR = nb(), nb(), nb()
                    for q4 in range(4):
                        tt = g * 4 + q4
                        ts_ = slice(tt * 128, (tt + 1) * 128)
                        qs = slice(q4 * 128, (q4 + 1) * 128)
                        c.ts("dve", dgb[g].v(None, dgb[g].h[:, qs]), id32, tcol("gcs", tt, h), ALU.mult)
                        yield
                        c.mm(pkk.v(None, pkk.h[:, qs]), kTn.v(tt, kTn.h[:, ts_]), kTn.v(tt, kTn.h[:, ts_]))
                        yield
                        c.mm(pqk.v(None, pqk.h[:, qs]), kTn.v(tt, kTn.h[:, ts_]), qTn.v(tt, qTn.h[:, ts_]))
                        yield
                        c.mm(pR.v(None, pR.h[:, qs]), ones, dgb[g].v(None, dgb[g].h[:, qs]))
                        yield
                    c.stt("dve", dcl[g][:, :], pR[:, :], -1.0, madd_low[:, :], ALU.mult, ALU.add)
                    yield
                    c.tt("dve", dcu[g][:, :], pR[:, :], madd_upi[:, :], ALU.add)
                    yield
                    for q4 in range(4):
                        tt = g * 4 + q4
                        qs = slice(q4 * 128, (q4 + 1) * 128)
                        c.act(dcl[g].v(None, dcl[g].h[:, qs]), dcl[g].v(None, dcl[g].h[:, qs]), AF.Exp, bias=tcol("gcs", tt, h))
                        yield
                        c.act(dcu[g].v(None, dcu[g].h[:, qs]), dcu[g].v(None, dcu[g].h[:, qs]), AF.Exp, bias=tcol("ngc", tt, h))
                        yield
                        c.stt("dve", Pb[g][0].v(None, Pb[g][0].h[:, qs]), pkk.v(None, pkk.h[:, qs]), tcol("nbeta", tt, h),
                              dcl[g].v(None, dcl[g].h[:, qs]), ALU.mult, ALU.mult)
                        yield
                    c.tt("dve", QKT.v(g, QKT.h[:, gs]), pqk[:, :], dcu[g][:, :], ALU.mult)
                    yield
                    pbt = nb()
                    for q4 in range(4):
                        qs = slice(q4 * 128, (q4 + 1) * 128)
                        c.tr(pbt.v(None, pbt.h[:, qs]), Pb[g][0].v(None, Pb[g][0].h[:, qs]), id32)
                        yield
                    c.cp("act", PTb[g][0][:, :], pbt[:, :])
                    yield
                    c.tt("dve", TT.v(g, TT.h[:, gs]), PTb[g][0][:, :], id4[:, :], ALU.add)
                    yield
                run_interleaved([prep(g) for g in grp])
                def sq_level(lev):
                    a, b2 = (lev - 1) % 2, lev % 2
                    outs = {}
                    for g in grp:
                        pP = nb()
                        for q4 in range(4):
                            qs = slice(q4 * 128, (q4 + 1) * 128)
                            c.mm(pP.v(None, pP.h[:, qs]), PTb[g][a].v(None, PTb[g][a].h[:, qs]), Pb[g][a].v(None, Pb[g][a].h[:, qs]))
                        pPT = None
                        if lev < 6:
                            pPT = nb()
                            for q4 in range(4):
                                qs = slice(q4 * 128, (q4 + 1) * 128)
                                c.mm(pPT.v(None, pPT.h[:, qs]), Pb[g][a].v(None, Pb[g][a].h[:, qs]), PTb[g][a].v(None, PTb[g][a].h[:, qs]))
                        outs[g] = (pP, pPT)
                    for g in grp:
                        pP, pPT = outs[g]
                        c.cp("act", Pb[g][b2][:, :], pP[:, :])
                        if pPT is not None:
                            c.cp("dve", PTb[g][b2][:, :], pPT[:, :])

                def t_level(lev):
                    b2 = lev % 2
                    outs = {}
                    for g in grp:
                        pT_ = nb()
                        for q4 in range(4):
                            qs = slice(q4 * 128, (q4 + 1) * 128)
                            tsl = slice(g * 512 + q4 * 128, g * 512 + (q4 + 1) * 128)
                            c.mm(pT_.v(None, pT_.h[:, qs]), Pb[g][b2].v(None, Pb[g][b2].h[:, qs]), TT.v(g, TT.h[:, tsl]))
                        outs[g] = pT_
                    for g in grp:
                        gs = slice(g * 512, (g + 1) * 512)
                        c.tt("dve", TT.v(g, TT.h[:, gs]), TT.v(g, TT.h[:, gs]), outs[g][:, :], ALU.add)

                sq_level(1)
                for lev in range(2, 7):
                    sq_level(lev)
                    t_level(lev - 1)
                t_level(6)
            A.reset(m_conv)
            rr = [A(f"rr{k}", [128, 128], F32) for k in range(2)]
            vn = [A(f"vn{k}", [128, 128], F32) for k in range(2)]
            qs_ = [A(f"qs{k}", [128, 128], F32) for k in range(2)]
            ob = [A(f"ob{k}", [128, 128], F32) for k in range(2)]
            osm = [A(f"osm{k}", [128, 4], F32) for k in range(2)]
            c.memset("dve", Sst[:, :], 0.0)

            def scan_R(tt):
                k = tt % 2
                ts_ = slice(tt * 128, (tt + 1) * 128)
                p1 = nb()
                c.mm(p1.v(None, p1.h[:, 0:128]), kTn.v(tt, kTn.h[:, ts_]), Sst[:, :])
                yield
                c.stt("dve", rr[k][:, :], p1.v(None, p1.h[:, 0:128]), tcol("nebg", tt, h), vb.v(tt, vb.h[:, tt, :]), ALU.mult, ALU.add)
                yield
                p2 = nb()
                c.mm(p2.v(None, p2.h[:, 0:128]), TT.v(tt // 4, TT.h[:, ts_]), rr[k][:, :])
                yield
                c.cp("act", vn[k][:, :], p2.v(None, p2.h[:, 0:128]))
                yield
                p3 = nb()
                c.mm(p3.v(None, p3.h[:, 0:128]), qTn.v(tt, qTn.h[:, ts_]), Sst[:, :])
                yield
                c.act(qs_[k][:, :], p3.v(None, p3.h[:, 0:128]), AF.Copy, scale=tcol("eg", tt, h))
                yield
                p5 = nb()
                c.mm(p5.v(None, p5.h[:, 0:128]), kdec.v(tt, kdec.h[:, tt, :]), vn[k][:, :])
                yield
                c.stt("dve", Sst[:, :], Sst[:, :], tcol("egl", tt, h), p5.v(None, p5.h[:, 0:128]), ALU.mult, ALU.add)
                yield

            def scan_O(tt):
                k = tt % 2
                ts_ = slice(tt * 128, (tt + 1) * 128)
                p4 = nb()
                c.mm(p4.v(None, p4.h[:, 0:128]), QKT.v(tt // 4, QKT.h[:, ts_]), vn[k][:, :])
                yield
                c.tt("dve", ob[k][:, :], p4.v(None, p4.h[:, 0:128]), qs_[k][:, :], ALU.add)
                yield
                o1 = osm[k]
                c.act(junk[:, :], ob[k][:, :], AF.Square, accum_out=o1.v(None, o1.h[:, 0:1]))
                yield
                c.ts("dve", o1.v(None, o1.h[:, 1:2]), o1.v(None, o1.h[:, 0:1]), 1.0 / 128.0, ALU.mult, 1e-6, ALU.add)
                yield
                c.act(o1.v(None, o1.h[:, 1:2]), o1.v(None, o1.h[:, 1:2]), AF.Sqrt)
                yield
                c.op("dve", lambda e, o1=o1: e.reciprocal(o1.h[:, 2:3], o1.h[:, 1:2]),
                     reads=[o1.v(None, o1.h[:, 1:2])], writes=[o1.v(None, o1.h[:, 2:3])])
                yield
                c.stt("dve", ob[k][:, :], ob[k][:, :], o1.v(None, o1.h[:, 2:3]), nw_bc[:, :], ALU.mult, ALU.mult)
                yield
                c.tt("dve", ob[k][:, :], ob[k][:, :], zall.v(tt, zall.h[:, tt, :]), ALU.mult)
                yield
                p6 = nb()
                c.tr(p6.v(None, p6.h[:, 0:128]), ob[k][:, :], id32)
                yield
                c.cp("act", dnT2[k][:, :], p6.v(None, p6.h[:, 0:128]))
                yield
                for half in range(2):
                    py = nb()
                    c.mm(py[:, :], dnT2[k][:, :], w_oB.v(None, w_oB.h[:, half * 512:(half + 1) * 512]))
                    yield
                    xh = x_cur.v(tt, x_cur.h[:, tt, half * 512:(half + 1) * 512])
                    c.tt("dve", xh, xh, py[:, :], ALU.add)
                    yield

            for tt in range(NT + 1):
                gens = []
                if tt < NT:
                    gens.append(scan_R(tt))
                if tt >= 1:
                    gens.append(scan_O(tt - 1))
                run_interleaved(gens)

    m_persist = A.mark()
    for l, (mixer, do_moe) in enumerate(cfg.layers):
        A.reset(m_persist)
        if mixer == "pool":
            pool_mixer(l // 2)
        elif mixer == "mix":
            for tt in range(NT):
                for half in range(2):
                    pb = PS[(2 * tt + half) % 8]
                    for q in range(4):
                        kc = half * 4 + q
                        c.tr(pb.v(None, pb.h[:, q * 128:(q + 1) * 128]), x_cur.v(tt, x_cur.h[:, tt, kc * 128:(kc + 1) * 128]), ident[:, :])
                    c.cp("act" if half == 0 else "dve", xT.v(tt, xT.h[:, half * 4:(half + 1) * 4, tt * 128:(tt + 1) * 128]),
                         V(pb, None, pb.h[:, :].rearrange("p (q n) -> p q n", q=4)))
            mixer0(l // 2)
        A.reset(m_persist)
        g_bc = A("g_bc", [128, D], F32)
        b_bc = A("b_bc", [128, D], F32)
        st = A("st", [128, 120], F32)
        mv = A("mv", [128, 40], F32)
        rstd = A("rstd", [128, 20], F32)
        m_mix = A.mark()
        if mixer != "none":
            bcast_load(g_bc[:, :], dr["ln1_g"][l:l + 1, :])
            bcast_load(b_bc[:, :], dr["ln1_b"][l:l + 1, :])
            layer_norm_all(g_bc, b_bc, (st, mv, rstd))
        A.reset(m_mix)
        w1u = [A(f"w1u{i_}", [128, KC, 512], BF16) for i_ in range(NBUF)]
        w2u = [A(f"w2u{i_}", [128, 2, D], BF16) for i_ in range(NBUF)]
        if "moe" not in cfg.skip:
            moe_load_unit(l, 0, w1u, w2u)
            moe_load_unit(l, 1, w1u, w2u)
        m_moe = A.mark()
        rw32 = A("rw32", [128, KC, E], F32)
        rb_bc = A("rb_bc", [128, E], F32)
        b2s = A("b2s", [E, D], F32)
        bufs = []
        for kb_ in range(10):
            bufs.append((A(f"xT32_{kb_}", [128, KC, 128], F32), A(f"lg{kb_}", [128, E], F32),
                         (A(f"m8_{kb_}", [128, 8], F32), A(f"negm{kb_}", [128, 1], F32), A(f"ex{kb_}", [128, E], F32), A(f"mask{kb_}", [128, E], F32),
                          A(f"ssum{kb_}", [128, 1], F32), A(f"rs{kb_}", [128, 1], F32)), A(f"gTs{kb_}", [E, 128], F32)))
        c.load("sp", rw32[:, :, :], [dr["router_w"][l].rearrange("(kc p) e -> p kc e", p=128)])
        bcast_load(rb_bc[:, :], dr["router_b"][l:l + 1, :])
        c.load("sp", b2s[:, :], [dr["moe_b2"][l, :, :]])
        if "post" not in cfg.skip:
            post_ln_all(l, rw32, rb_bc, b2s, bufs)
        if "moe" not in cfg.skip:
            moe(l, m_moe, w1u, w2u)
        bcast_load(g_bc[:, :], dr["ln2_g"][l:l + 1, :])
        bcast_load(b_bc[:, :], dr["ln2_b"][l:l + 1, :])
        last = (l == len(cfg.layers) - 1)
        ov = out_d.rearrange("(t p) d -> p t d", p=128)
        st_fn = (lambda tt: c.store("sp", [ov[:, tt, :]], x_cur.v(tt, x_cur.h[:, tt, :]))) if last else None
        if "ln2" not in cfg.skip:
            layer_norm_all(g_bc, b_bc, (st, mv, rstd), after=st_fn)
        elif last:
            for tt in range(NT):
                st_fn(tt)
    c.emit()
    return nc, c


def host_consts():
    p = np.arange(128)
    ident = np.eye(128, dtype=np.float32)
    bands = np.zeros((128, 12, 128), np.float32)
    s = p[:, None]
    t = p[None, :]
    for g, w in enumerate((2, 4, 8, 16)):
        d = t - s
        cur = np.where((d >= 0) & (d < w), 1.0 / w, 0.0) - (d == 0)
        cnt = np.minimum(t + 1, w)
        cur0 = np.where((d >= 0) & (d < w), 1.0 / cnt, 0.0) - (d == 0)
        dprev = t + 128 - s
        prev = np.where(dprev < w, 1.0 / w, 0.0)
        bands[:, 3 * g + 0, :] = cur0
        bands[:, 3 * g + 1, :] = cur
        bands[:, 3 * g + 2, :] = prev
    cm = np.zeros((128, NCM), np.float32)
    cm[:, 0:128] = (s <= t)
    cm[:, 128:256] = 1.0
    P = np.zeros((128, 128), np.float32)
    for base in (0, 64):
        for m in range(8):
            P[base + m + 8, base + m] = -1.0
        for m in range(8, 16):
            P[base + m - 8, base + m] = 1.0
    cm[:, 256:384] = P
    cm[:, 384:512] = (s > t)
    cm[:, 512:640] = (t >= s)
    qi = p[:, None]
    kj = np.arange(256)[None, :]
    valid = np.where(kj < 128, kj > qi, (kj - 128) <= qi)
    cm[:, 640:896] = np.where(valid, 0.0, -30000.0)
    half = 8
    inv = 500000.0 ** (-np.arange(half, dtype=np.float32) / half)
    f = np.zeros(128, np.float32)
    for base in (0, 64):
        for d in range(16):
            f[base + d] = inv[d % 8]
    cm[:, 896] = f
    cm[:, 897:1025] = ident
    return dict(ident=ident, bands=bands, cmix=cm)


_CACHE = {}


def kernel(**inputs):
    from concourse.bass_utils import run_bass_kernel_spmd
    B = inputs["x"].shape[0]
    if "nc" not in _CACHE:
        cfg = Cfg(T=2048, E=32, layers=(("mix", True), ("pool", True)))
        _CACHE["nc"] = build(cfg)[0]
    nc = _CACHE["nc"]
    hc = host_consts()
    f32 = lambda a: np.ascontiguousarray(np.asarray(a), dtype=np.float32)
    shared = {k: f32(inputs[k]) for k in ("ln1_g", "ln1_b", "ln2_g", "ln2_b", "router_w", "router_b", "moe_w1", "moe_b1",
                                           "moe_w2", "moe_b2", "pool_w", "pool_scale", "mix_w_in", "mix_b_in", "dn_conv_w",
                                           "dn_a_log", "dn_dt_bias", "dn_norm_w", "swa_sinks", "mix_w_out", "mix_b_out")}
    shared["pool_b"] = f32(inputs["pool_b"]).reshape(1, D)
    shared.update(hc)
    x = f32(inputs["x"])
    pos = np.ascontiguousarray(np.asarray(inputs["positions"]), dtype=np.int32)
    in_maps = []
    for b in range(B):
        m = dict(shared)
        m["x"] = x[b]
        m["positions"] = pos[b:b + 1]
        in_maps.append(m)
    res = run_bass_kernel_spmd(nc, in_maps, core_ids=list(range(B)))
    return np.stack([np.asarray(r["out"], dtype=np.float32) for r in res.results], axis=0)
```
